# Optimizing a Trainium2 kernel written in Bass

```python
import jax, jax.numpy as jnp
from jax import lax
import numpy as np

D_MODEL = 1024
BATCH = 8
SEQ = 4096
DEPTH = 2

GLA_HEADS = 4
GLA_KEY_DIM = D_MODEL // 2
GLA_VAL_DIM = D_MODEL
GLA_HEAD_K = GLA_KEY_DIM // GLA_HEADS
GLA_HEAD_V = GLA_VAL_DIM // GLA_HEADS
GLA_GATE_RANK = 16
GLA_GATE_NORMALIZER = 16.0
GLA_CHUNK = 64
GLA_NORM_EPS = 1e-5
CONV_CH = D_MODEL
CONV_WIDTH = 31
LN_EPS = 1e-5
POOL_WINDOWS = (2, 4, 8, 16)
POOL_GROUPS = 4
POOL_CH = D_MODEL
POOL_GROUP_CH = POOL_CH // POOL_GROUPS
N_BRANCH = 3
IN_SPLITS = (GLA_KEY_DIM, GLA_KEY_DIM, GLA_VAL_DIM, GLA_VAL_DIM, GLA_GATE_RANK, 2 * CONV_CH, POOL_CH, N_BRANCH * D_MODEL)
IN_DIM = int(sum(IN_SPLITS))
IN_OFFSETS = [int(o) for o in np.cumsum(IN_SPLITS)[:-1]]
FFN_DIM = ((8 * D_MODEL // 3 + 255) // 256) * 256
N_EXPERTS = 8
TOP_K = 2
EXPERT_DIM = 7 * D_MODEL // 2
MOE_BLOCK = 256
PLE_DIM = 256
RMS_EPS = 1e-6

kernel_name = 'hybrid_gla_conformer_pool_moe_trunk'


def rms_norm(x, g):
    x32 = x.astype(jnp.float32)
    y = x32 * lax.rsqrt(jnp.mean(x32 * x32, axis=-1, keepdims=True) + RMS_EPS)
    return (y * g.astype(jnp.float32)).astype(x.dtype)


def swiglu(h, w_gate, w_up, w_down):
    return (jax.nn.silu(h @ w_gate) * (h @ w_up)) @ w_down


def gla_chunked(q, k, v, gk):
    B_, S_, H, DK = q.shape
    DV = v.shape[-1]
    C = GLA_CHUNK
    N = S_ // C

    def chunks(t):
        return t.reshape(B_, N, C, H, t.shape[-1]).transpose(1, 0, 3, 2, 4)

    q = chunks(q * (DK ** -0.5))
    k = chunks(k)
    v = chunks(v)
    b = jnp.cumsum(chunks(gk), axis=3)
    b_last = b[:, :, :, C - 1:C, :]
    b_mid = b[:, :, :, C // 2 - 1:C // 2, :]
    causal = jnp.tril(jnp.ones((C, C), dtype=bool))
    attn = jnp.einsum('nbhid,nbhjd->nbhij', q * jnp.exp(b - b_mid), k * jnp.exp(b_mid - b))
    attn = jnp.where(causal, attn, 0.0)
    o_intra = jnp.einsum('nbhij,nbhjv->nbhiv', attn, v)
    q_dec = q * jnp.exp(b)
    k_dec = k * jnp.exp(b_last - b)
    chunk_decay = jnp.exp(b_last[:, :, :, 0, :])

    def step(state, xs):
        qd, kd, vc, dec = xs
        o = jnp.einsum('bhid,bhdv->bhiv', qd, state)
        state = state * dec[..., None] + jnp.einsum('bhjd,bhjv->bhdv', kd, vc)
        return state, o

    state0 = jnp.zeros((B_, H, DK, DV), jnp.float32)
    _, o_inter = lax.scan(step, state0, (q_dec, k_dec, v, chunk_decay))
    o = o_intra + o_inter
    return o.transpose(1, 0, 3, 2, 4).reshape(B_, S_, H, DV)


def conformer_conv(u2, conv_w, conv_b, ln_g, ln_b, w_proj):
    a, g = jnp.split(u2, 2, axis=-1)
    u = a * jax.nn.sigmoid(g)
    y = lax.conv_general_dilated(u, conv_w[:, None, :], window_strides=(1,), padding=[(CONV_WIDTH - 1, 0)], dimension_numbers=('NWC', 'WIO', 'NWC'), feature_group_count=CONV_CH) + conv_b
    y32 = y.astype(jnp.float32)
    mu = jnp.mean(y32, axis=-1, keepdims=True)
    var = jnp.mean(jnp.square(y32 - mu), axis=-1, keepdims=True)
    yn = (y32 - mu) * lax.rsqrt(var + LN_EPS) * ln_g.astype(jnp.float32) + ln_b.astype(jnp.float32)
    return jax.nn.silu(yn).astype(u2.dtype) @ w_proj


def multiscale_pool(u, w_pool, scale):
    B_, S_, _ = u.shape
    ug = u.astype(jnp.float32).reshape(B_, S_, POOL_GROUPS, POOL_GROUP_CH)
    cs = jnp.concatenate([jnp.zeros((B_, 1, POOL_GROUPS, POOL_GROUP_CH), jnp.float32), jnp.cumsum(ug, axis=1)], axis=1)
    t = jnp.arange(S_)
    means = []
    for gi, w in enumerate(POOL_WINDOWS):
        lo = jnp.maximum(t + 1 - w, 0)
        wsum = cs[:, 1:, gi] - jnp.take(cs[:, :, gi], lo, axis=1)
        cnt = jnp.minimum(t + 1, w).astype(jnp.float32)
        means.append(wsum / cnt[None, :, None])
    d = (jnp.stack(means, axis=2) - ug).astype(u.dtype)
    y = jnp.einsum('bsgc,gcd->bsgd', d, w_pool).reshape(B_, S_, POOL_CH)
    return y * scale


def moe_swiglu(h, w_router, b_router, w_gate, w_up, w_down):
    B_, S_, D_ = h.shape
    T = B_ * S_
    xt = h.reshape(T, D_)
    logits = (xt @ w_router).astype(jnp.float32) + b_router.astype(jnp.float32)
    top_logit, top_idx = lax.top_k(logits, TOP_K)
    top_w = jax.nn.softmax(top_logit, axis=-1)
    A = T * TOP_K
    flat_e = top_idx.reshape(A).astype(jnp.int32)
    flat_tok = jnp.repeat(jnp.arange(T, dtype=jnp.int32), TOP_K)
    flat_w = top_w.reshape(A)
    order = jnp.argsort(flat_e, stable=True)
    e_sorted = flat_e[order]
    tok_sorted = flat_tok[order]
    w_sorted = flat_w[order]
    counts = jnp.zeros((N_EXPERTS,), jnp.int32).at[flat_e].add(1)
    padded = (counts + MOE_BLOCK - 1) // MOE_BLOCK * MOE_BLOCK
    pad_end = jnp.cumsum(padded)
    pad_start = pad_end - padded
    start = jnp.cumsum(counts) - counts
    dest = pad_start[e_sorted] + jnp.arange(A, dtype=jnp.int32) - start[e_sorted]
    n_blocks = -(-A // MOE_BLOCK) + N_EXPERTS
    rows = jnp.full((n_blocks * MOE_BLOCK,), T, jnp.int32).at[dest].set(tok_sorted)
    block_start = jnp.arange(n_blocks, dtype=jnp.int32) * MOE_BLOCK
    block_e = jnp.minimum(jnp.searchsorted(pad_end, block_start, side='right'), N_EXPERTS - 1)
    x_pad = jnp.concatenate([xt, jnp.zeros((1, D_), xt.dtype)], axis=0)

    def expert_block(args):
        r, e = args
        return swiglu(x_pad[r], w_gate[e], w_up[e], w_down[e])

    yb = lax.map(expert_block, (rows.reshape(n_blocks, MOE_BLOCK), block_e))
    y_sorted = yb.reshape(n_blocks * MOE_BLOCK, D_)[dest]
    y = jax.ops.segment_sum(y_sorted * w_sorted[:, None].astype(y_sorted.dtype), tok_sorted, num_segments=T)
    return y.reshape(B_, S_, D_)


def setup_inputs(seed: int = 0) -> dict:
    key = jax.random.key(seed)
    keys = jax.random.split(key, 40)
    ctr = [0]

    def nk():
        ctr[0] += 1
        return keys[ctr[0] - 1]

    def nrm(shape, scale):
        return jax.random.normal(nk(), shape, jnp.float32) * scale

    def gain(shape):
        return 1.0 + 0.05 * jax.random.normal(nk(), shape, jnp.float32)

    nd = (DEPTH + 1) // 2
    nm = DEPTH // 2
    return {
        'x': nrm((BATCH, SEQ, D_MODEL), 1.0),
        'p': nrm((DEPTH, BATCH, SEQ, PLE_DIM), 1.0),
        'g_mix': gain((DEPTH, D_MODEL)),
        'w_in': nrm((DEPTH, D_MODEL, IN_DIM), D_MODEL ** -0.5),
        'w_gk_up': nrm((DEPTH, GLA_GATE_RANK, GLA_KEY_DIM), GLA_GATE_RANK ** -0.5),
        'b_gk': nrm((DEPTH, GLA_KEY_DIM), 0.1),
        'g_gla_norm': gain((DEPTH, GLA_HEAD_V)),
        'conv_w': nrm((DEPTH, CONV_WIDTH, CONV_CH), CONV_WIDTH ** -0.5),
        'conv_b': nrm((DEPTH, CONV_CH), 0.02),
        'ln_conv_g': gain((DEPTH, CONV_CH)),
        'ln_conv_b': nrm((DEPTH, CONV_CH), 0.02),
        'w_conv_out': nrm((DEPTH, CONV_CH, D_MODEL), CONV_CH ** -0.5),
        'w_pool': nrm((DEPTH, POOL_GROUPS, POOL_GROUP_CH, POOL_GROUP_CH), POOL_GROUP_CH ** -0.5),
        'pool_scale': gain((DEPTH, POOL_CH)),
        'w_out': nrm((DEPTH, D_MODEL, D_MODEL), D_MODEL ** -0.5),
        'g_ffn': gain((DEPTH, D_MODEL)),
        'w_ffn_gate': nrm((nd, D_MODEL, FFN_DIM), D_MODEL ** -0.5),
        'w_ffn_up': nrm((nd, D_MODEL, FFN_DIM), D_MODEL ** -0.5),
        'w_ffn_down': nrm((nd, FFN_DIM, D_MODEL), FFN_DIM ** -0.5),
        'w_router': nrm((nm, D_MODEL, N_EXPERTS), D_MODEL ** -0.5),
        'b_router': nrm((nm, N_EXPERTS), 0.01),
        'w_moe_gate': nrm((nm, N_EXPERTS, D_MODEL, EXPERT_DIM), D_MODEL ** -0.5),
        'w_moe_up': nrm((nm, N_EXPERTS, D_MODEL, EXPERT_DIM), D_MODEL ** -0.5),
        'w_moe_down': nrm((nm, N_EXPERTS, EXPERT_DIM, D_MODEL), EXPERT_DIM ** -0.5),
        'g_ple': gain((DEPTH, D_MODEL)),
        'w_ple_gate': nrm((DEPTH, D_MODEL, D_MODEL), D_MODEL ** -0.5),
        'w_ple_proj': nrm((DEPTH, PLE_DIM, D_MODEL), PLE_DIM ** -0.5),
        'g_final': gain((D_MODEL,)),
    }


def reference(x, p, g_mix, w_in, w_gk_up, b_gk, g_gla_norm, conv_w, conv_b, ln_conv_g, ln_conv_b, w_conv_out, w_pool, pool_scale, w_out, g_ffn, w_ffn_gate, w_ffn_up, w_ffn_down, w_router, b_router, w_moe_gate, w_moe_up, w_moe_down, g_ple, w_ple_gate, w_ple_proj, g_final):
    B_, S_, _ = x.shape
    f32 = jnp.float32
    for i in range(DEPTH):
        h = rms_norm(x, g_mix[i])
        z = h @ w_in[i]
        q_in, k_in, v_in, g_in, gk_lr, conv_in, pool_in, gate_in = jnp.split(z, IN_OFFSETS, axis=-1)

        gk = jax.nn.log_sigmoid((gk_lr @ w_gk_up[i]).astype(f32) + b_gk[i].astype(f32)) / GLA_GATE_NORMALIZER
        o = gla_chunked(q_in.astype(f32).reshape(B_, S_, GLA_HEADS, GLA_HEAD_K), k_in.astype(f32).reshape(B_, S_, GLA_HEADS, GLA_HEAD_K), v_in.astype(f32).reshape(B_, S_, GLA_HEADS, GLA_HEAD_V), gk.reshape(B_, S_, GLA_HEADS, GLA_HEAD_K))
        o = o * lax.rsqrt(jnp.mean(o * o, axis=-1, keepdims=True) + GLA_NORM_EPS) * g_gla_norm[i].astype(f32)
        o = o * jax.nn.silu(g_in.astype(f32).reshape(B_, S_, GLA_HEADS, GLA_HEAD_V))
        y_gla = o.reshape(B_, S_, GLA_VAL_DIM)

        y_conv = conformer_conv(conv_in, conv_w[i], conv_b[i], ln_conv_g[i], ln_conv_b[i], w_conv_out[i]).astype(f32)

        y_pool = multiscale_pool(pool_in, w_pool[i], pool_scale[i]).astype(f32)

        gates = jax.nn.sigmoid(gate_in.astype(f32)).reshape(B_, S_, N_BRANCH, D_MODEL)
        mix = gates[:, :, 0] * y_gla + gates[:, :, 1] * y_conv + gates[:, :, 2] * y_pool
        x = x + mix.astype(x.dtype) @ w_out[i]

        h = rms_norm(x, g_ffn[i])
        if i % 2 == 0:
            y = swiglu(h, w_ffn_gate[i // 2], w_ffn_up[i // 2], w_ffn_down[i // 2])
        else:
            y = moe_swiglu(h, w_router[i // 2], b_router[i // 2], w_moe_gate[i // 2], w_moe_up[i // 2], w_moe_down[i // 2])
        x = x + y

        hp = rms_norm(x, g_ple[i])
        x = x + jax.nn.sigmoid(hp @ w_ple_gate[i]) * (p[i] @ w_ple_proj[i])
    return rms_norm(x, g_final)
```

```python
import numpy as np
from contextlib import ExitStack
import concourse.bass as bass
import concourse.mybir as mybir
from concourse.bass_utils import run_bass_kernel_spmd

F32 = mybir.dt.float32
BF16 = mybir.dt.bfloat16
AF = mybir.ActivationFunctionType
ALU = mybir.AluOpType
AX = mybir.AxisListType

_DT_SIZE = {F32: 4, BF16: 2}
EPOCH = 20000

SEQ = 4096
D = 1024
NL = 2
IN_DIM = 9232
ST = 512
NST = SEQ // ST
FFN = 2816
NE = 8
EXP = 3584
XPREF = True
LOOK = 2
LOOK2 = 2


class _Op:
    __slots__ = ("eng", "idx", "fn", "deps", "signal", "sigcount", "dma", "fill_end", "prefill")

    def __init__(self, eng, idx, fn):
        self.eng = eng
        self.idx = idx
        self.fn = fn
        self.deps = []
        self.signal = False
        self.sigcount = 0
        self.dma = None
        self.fill_end = 0
        self.prefill = 0


class DmaStream:
    def __init__(self, name):
        self.name = name
        self.count = 0
        self.sem = None


class SemPool:
    def __init__(self, nc):
        self.nc = nc
        self.es = ExitStack()
        self.eng = {}
        self.free = {"sp": [], "pool": [], "act": []}
        self.n = 0

    def new(self, tag):
        self.n += 1
        return self.es.enter_context(self.nc.semaphore(f"s{self.n}_{tag}"))

    def eng_sem(self, e, idx):
        lst = self.eng.setdefault(e, [[], 0])[0]
        while len(lst) <= idx:
            lst.append(self.new(e))
        return lst[idx]

    def take(self, q):
        if self.free[q]:
            return self.free[q].pop()
        return [self.new("d" + q), 0]

    def give(self, q, ent):
        self.free[q].append(ent)


class Phase:
    ENGS = ("pe", "act", "dve", "pool", "sp")

    def __init__(self, nc, name="ph"):
        self.nc = nc
        self.name = name
        self.ops = {e: [] for e in self.ENGS}
        self.track = {}
        self.streams = []
        self.notrack = set()
        self.pool = nc._sempool

    def _rng(self, ap):
        name = ap.tensor.name
        apl = ap.ap
        off = ap.offset
        ds = _DT_SIZE.get(ap.dtype, 4)
        space = str(ap.space)
        if space in ("SB", "PSUM"):
            pstep, pcnt = apl[0]
            if pstep == 0:
                p0 = 0
                f0 = off
            else:
                p0 = off // pstep
                f0 = off - p0 * pstep
            ext = 0
            for st, cn in apl[1:]:
                ext += abs(st) * (cn - 1)
            lo, hi = f0 * ds, (f0 + ext + 1) * ds
            if space == "PSUM":
                return name, 0, 128, (lo // 2048) * 2048, ((hi + 2047) // 2048) * 2048
            return name, p0, p0 + pcnt, lo, hi
        ext = 0
        for st, cn in apl:
            ext += abs(st) * (cn - 1)
        return name, 0, 1, off * ds, (off + ext + 1) * ds

    def _access(self, op, ap, write):
        name, p0, p1, lo, hi = self._rng(ap)
        if name in self.notrack:
            return
        recs = self.track.get(name)
        if recs is None:
            recs = self.track[name] = []
        newrecs = []
        for r in recs:
            rp0, rp1, rlo, rhi, rop, rw = r
            if rp1 <= p0 or p1 <= rp0 or rhi <= lo or hi <= rlo:
                newrecs.append(r)
                continue
            if (write or rw) and rop is not op:
                op.deps.append(rop)
            covered = (p0 <= rp0 and rp1 <= p1 and lo <= rlo and rhi <= hi)
            if write and covered:
                continue
            if (not write) and (not rw) and covered and rop.eng == op.eng and rop.dma is None and op.dma is None:
                continue
            newrecs.append(r)
        newrecs.append((p0, p1, lo, hi, op, write))
        self.track[name] = newrecs

    def _add(self, eng, fn, reads, writes):
        lst = self.ops[eng]
        op = _Op(eng, len(lst), fn)
        lst.append(op)
        for ap in reads:
            if ap is not None and not isinstance(ap, (int, float)):
                self._access(op, ap, False)
        for ap in writes:
            self._access(op, ap, True)
        return op

    def mm(self, out, lhsT, rhs, start=True, stop=True):
        return self._add("pe", lambda e: e.matmul(out, lhsT, rhs, start=start, stop=stop), [lhsT, rhs], [out])

    def transpose(self, out, in_, ident):
        return self._add("pe", lambda e: e.transpose(out, in_, ident), [in_, ident], [out])

    def act(self, out, in_, func, bias=None, scale=None, accum_out=None):
        kw = {}
        if bias is not None:
            kw["bias"] = bias
        if scale is not None:
            kw["scale"] = scale
        if accum_out is not None:
            kw["accum_out"] = accum_out
        w = [out] + ([accum_out] if accum_out is not None else [])
        return self._add("act", lambda e: e.activation(out, in_, func, **kw), [in_, bias, scale], w)

    def tt(self, eng, out, in0, in1, op):
        return self._add(eng, lambda e: e.tensor_tensor(out, in0, in1, op), [in0, in1], [out])

    def ts(self, eng, out, in0, s1, s2, op0, op1=None):
        if op1 is None:
            return self._add(eng, lambda e: e.tensor_scalar(out, in0, s1, None, op0), [in0, s1], [out])
        return self._add(eng, lambda e: e.tensor_scalar(out, in0, s1, s2, op0, op1), [in0, s1, s2], [out])

    def stt(self, eng, out, in0, scalar, in1, op0, op1):
        return self._add(eng, lambda e: e.scalar_tensor_tensor(out, in0, scalar, in1, op0, op1),
                         [in0, scalar, in1], [out])

    def copy(self, eng, out, in_):
        if eng == "act":
            return self._add("act", lambda e: e.copy(out, in_), [in_], [out])
        return self._add(eng, lambda e: e.tensor_copy(out, in_), [in_], [out])

    def recip(self, out, in_):
        return self._add("dve", lambda e: e.reciprocal(out, in_), [in_], [out])

    def memset(self, eng, ap, val):
        return self._add(eng, lambda e: e.memset(ap, val), [], [ap])

    def reduce_max(self, out, in_):
        return self._add("dve", lambda e: e.tensor_reduce(out, in_, AX.X, ALU.max), [in_], [out])

    def stream(self, name):
        s = DmaStream(name)
        self.streams.append(s)
        return s

    def dma(self, q, stream, pairs):
        prefill = stream.count
        ops = []
        for (out, in_) in pairs:
            op = self._add(q, (lambda e, out=out, in_=in_: e.dma_start(out, in_)), [in_], [out])
            op.dma = stream
            op.prefill = prefill
            stream.count += 1
            ops.append(op)
        for op in ops:
            op.fill_end = stream.count
        return ops

    def emit(self, es):
        nc = self.nc
        pool = self.pool
        fin = _Op("sp", len(self.ops["sp"]), None)
        self.ops["sp"].append(fin)

        def needs_sem(op, d):
            if d.dma is not None:
                return False
            if d.eng == op.eng and op.eng == "pe":
                return False
            return True

        for e in self.ENGS:
            for op in self.ops[e]:
                for d in op.deps:
                    if needs_sem(op, d):
                        d.signal = True
        for e in self.ENGS:
            ent = pool.eng.setdefault(e, [[], 0])
            c = ent[1]
            for op in self.ops[e]:
                if op.signal and op.dma is None:
                    c += 1
                    op.sigcount = c
            ent[1] = c

        def esem(e, c):
            c0 = c - 1
            return pool.eng_sem(e, c0 // EPOCH), (c0 % EPOCH) + 1
        sq = {}
        for e in self.ENGS:
            for op in self.ops[e]:
                if op.dma is not None:
                    assert sq.setdefault(op.dma, e) == e, "a DMA stream must stay on one queue"
        for st_ in self.streams:
            if st_ in sq:
                st_.ent = pool.take(sq[st_])
                st_.sem = st_.ent[0]
                st_.base = st_.ent[1]
        streams = [st_ for st_ in self.streams if st_ in sq]
        ops_all = self.ops

        def run(ename, eng):
            waited = {}

            def wait(sem, val):
                k = id(sem)
                if waited.get(k, 0) >= val:
                    return
                waited[k] = val
                eng.wait_ge(sem, val)

            for op in ops_all[ename]:
                if op.fn is None:
                    for st_ in streams:
                        if st_.count:
                            wait(st_.sem, 16 * (st_.base + st_.count))
                    continue
                for d in op.deps:
                    if d.dma is not None:
                        wait(d.dma.sem, 16 * (d.dma.base + d.fill_end))
                    elif needs_sem(op, d):
                        wait(*esem(d.eng, d.sigcount))
                if op.dma is not None and op.prefill > 0:
                    wait(op.dma.sem, 16 * (op.dma.base + op.prefill))
                ins = op.fn(eng)
                if op.dma is not None:
                    ins.then_inc(op.dma.sem, 16)
                elif op.signal:
                    ins.then_inc(esem(ename, op.sigcount)[0], 1)

        with nc.Block() as block:
            @block.tensor
            def _(e):
                run("pe", e)

            @block.scalar
            def _(e):
                run("act", e)

            @block.vector
            def _(e):
                run("dve", e)

            @block.gpsimd
            def _(e):
                run("pool", e)

            @block.sync
            def _(e):
                run("sp", e)
        for st_ in streams:
            st_.ent[1] = st_.base + st_.count
            pool.give(sq[st_], st_.ent)


OQ, OK_, OV, OG, OGK, OCA, OCG, OPL, OG0, OG1, OG2 = 0, 512, 1024, 2048, 3072, 3088, 4112, 5136, 6160, 7184, 8208
VF_CW, VF_CB, VF_LG, VF_LB, VF_PS, VF_GN, VF_N = 0, 248, 256, 264, 272, 280, 282
VT_GMIX, VT_GFFN, VT_GPLE, VT_GFIN, VT_N = 0, 2, 4, 6, 7


class Ctx:
    pass


class SlabQ:
    def __init__(self, P, w_in, bufs, streams, cols, look=2):
        self.P, self.w_in, self.bufs, self.streams, self.cols, self.look = P, w_in, bufs, streams, cols, look
        self.issued = 0
        self.used = 0
        self.after = None

    def _issue(self):
        i = self.issued
        b = i % len(self.bufs)
        c0 = self.cols[i]
        self.P.dma("pool", self.streams[b],
                   [(self.bufs[b][:], self.w_in[:, c0:c0 + 512].rearrange("(k p) n -> p k n", p=128))])
        self.issued += 1
        if self.after is not None and self.issued == self.after[0]:
            self.after[1]()

    def get(self):
        while self.issued < len(self.cols) and self.issued <= self.used + self.look:
            self._issue()
        b = self.used % len(self.bufs)
        self.used += 1
        return self.bufs[b]


def rms_to_hT(P, K, xt, gb, hb, hT, pT, identb, ss, sd, junk, ntc=4, nhb=4):
    for tc in range(ntc):
        P.act(junk[:], xt[:, tc, :], AF.Square, scale=1.0 / 32.0, accum_out=ss[:, tc:tc + 1])
    P.act(sd[:, 0:ntc], ss[:, 0:ntc], AF.Sqrt, bias=K.eps6[:, 0:1])
    P.recip(sd[:, 0:ntc], sd[:, 0:ntc])
    for tc in range(ntc):
        hbt = hb[:, tc % nhb, :]
        P.stt("dve", hbt, xt[:, tc, :], sd[:, tc:tc + 1], gb[:], ALU.mult, ALU.mult)
        for kc in range(8):
            P.transpose(pT[:, kc, :], hbt[:, kc * 128:(kc + 1) * 128], identb[:])
        P.copy("act", hT[:, :, tc * 128:(tc + 1) * 128], pT[:])


def phase_m1(K, li, x_src):
    nc = K.nc
    with ExitStack() as es:
        P = Phase(nc, f"m1{li}")
        pre = f"m1{li}_"
        sb = lambda n, s, d: es.enter_context(nc.sbuf_tensor(pre + n, s, d))
        ps = lambda n, s, d: es.enter_context(nc.psum_tensor(pre + n, s, d))
        pO = ps("pO", [128, 8, 128], F32)
        pS = ps("pS", [128, 4, 256], F32)
        pT = ps("pT", [128, 8, 128], BF16)
        gps = [ps(f"g{i}", [128, 512], F32) for i in range(3)]
        gi = [0]

        def gp():
            gi[0] += 1
            return gps[gi[0] % 3]
        cst = sb("cst", [128, 3, 128], F32)
        identb = sb("identb", [128, 128], BF16)
        onesb = sb("onesb", [128, 128], BF16)
        K.eps6 = sb("eps6", [128, 1], F32)
        eps5 = sb("eps5", [128, 1], F32)
        gb = sb("gb", [128, 1024], F32)
        wgk = sb("wgk", [17, 512], BF16)
        wgkl = sb("wgkl", [128, 8, 16], BF16)
        vf = sb("vf", [128, VF_N], F32)
        S = sb("S", [128, 4, 256], F32)
        Sb = sb("Sb", [128, 4, 256], BF16)
        gkl = sb("gkl", [17, 512], BF16)
        xt = sb("xt", [128, 4, 1024], F32)
        junk = sb("junk", [128, 1024], BF16)
        ss = sb("ss", [128, 4], F32)
        sd = sb("sd", [128, 4], F32)
        hb = sb("hb", [128, 4, 1024], BF16)
        hT = sb("hT", [128, 8, 512], BF16)
        NB = 3
        wsl = [sb(f"wsl{i}", [128, 8, 512], BF16) for i in range(NB)]
        q_fm = sb("q_fm", [128, 4, 512], F32)
        k_fm = sb("k_fm", [128, 4, 512], F32)
        k_tm = sb("k_tm", [128, 4, 512], F32)
        v_tm = sb("v_tm", [128, 4, 1024], BF16)
        sp_tm = sb("sp_tm", [128, 4, 512], F32)
        e1 = sb("e1", [128, 512], F32)
        kdec = sb("kdec", [128, 4, 512], BF16)
        B_sb = sb("B_sb", [128, 4, 128], F32)
        Dm = sb("Dm", [128, 4, 128], F32)
        ea = sb("ea", [128, 4, 128], F32)
        eb = sb("eb", [128, 4, 128], F32)
        ec = sb("ec", [128, 4, 128], F32)
        qi = sb("qi", [128, 4, 128], BF16)
        ki = sb("ki", [128, 4, 128], BF16)
        qd = sb("qd", [128, 4, 128], BF16)
        attn = sb("attn", [128, 4, 128], BF16)
        o_sb = sb("o_sb", [128, 8, 512], F32)
        osq = sb("osq", [128, 8, 512], BF16)
        gs = sb("gs", [128, 8, 512], BF16)
        rn = sb("rn", [128, 4, 512], F32)
        t1s = [sb(f"t1{i}", [128, 512], F32) for i in range(2)]
        sg8 = sb("sg8", [128, 8, 512], BF16)
        mix = sb("mix", [128, 8, 512], F32)
        s_c = P.stream("c")
        s_cp = P.stream("cp")
        s_x = P.stream("x")
        s_w = [P.stream(f"w{i}") for i in range(NB)]
        s_h = P.stream("h")
        s_m = P.stream("m")

        ident = cst[:, 0, :]
        U = cst[:, 1, :]
        SL = cst[:, 2, :]
        P.dma("sp", s_c, [(cst[:], K.consts[0:3].rearrange("c p n -> p c n")),
                          (gb[:], K.vtm[VT_GMIX + li].partition_broadcast(128)),
                          (vf[:], K.vfm[li])])
        P.dma("pool", s_cp, [(wgk[:], K.wgk[li]),
                            (wgkl[:], K.w_in[li][:, OGK:OGK + 16].rearrange("(k p) n -> p k n", p=128))])
        P.copy("dve", identb[:], ident)
        P.memset("dve", onesb[:], 1.0)
        P.memset("dve", K.eps6[:], 1e-6)
        P.memset("dve", eps5[:], 1e-5)
        P.memset("dve", S[:], 0.0)
        P.memset("pool", Sb[:], 0.0)
        P.memset("pool", gkl[:], 1.0)

        slab_cols = [OQ, OK_, OV, OV + 512, OG, OG + 512, OG0, OG0 + 512]
        w_in = K.w_in[li]
        sq = SlabQ(P, w_in, wsl, s_w, slab_cols * NST, look=LOOK)

        def load_slab(c0):
            assert sq.cols[sq.used] == c0
            return sq.get()

        def load_x(st_):
            P.dma("sp", s_x, [(xt[:], x_src[st_ * ST:(st_ + 1) * ST, :].rearrange("(c p) d -> p c d", p=128))])

        def proj_fm(w, c0, out_ps):
            for kc in range(8):
                P.mm(out_ps, w[:, kc, c0:c0 + 128], hT[:, kc, :], start=(kc == 0), stop=(kc == 7))

        def proj_tm(w, tc, out_ps, n=512):
            for kc in range(8):
                P.mm(out_ps, hT[:, kc, tc * 128:(tc + 1) * 128], w[:, kc, 0:n], start=(kc == 0), stop=(kc == 7))

        hT_dv = K.hT_d.rearrange("k p t -> p k t")
        mix_dv = K.mix_d.rearrange("c p t -> p c t")
        def tail_a():
            P.act(gs[:], gs[:], AF.Silu)
            P.act(sg8[:], sg8[:], AF.Sigmoid)
            for c in range(8):
                t1_ = t1s[c % 2]
                P.tt("pool", osq[:, c, :], o_sb[:, c, :], o_sb[:, c, :], ALU.mult)
                P.stt("dve", t1_[:], o_sb[:, c, :], vf[:, VF_GN + (c % 2):VF_GN + (c % 2) + 1], gs[:, c, :],
                      ALU.mult, ALU.mult)
                P.tt("pool", mix[:, c, :], t1_[:], sg8[:, c, :], ALU.mult)

        def tail_b(t0):
            for h in range(4):
                g = gp()
                P.mm(g[:], onesb[:], osq[:, 2 * h, :], start=True, stop=False)
                P.mm(g[:], onesb[:], osq[:, 2 * h + 1, :], start=False, stop=True)
                P.act(rn[:, h, :], g[:], AF.Sqrt, bias=eps5[:, 0:1], scale=1.0 / 256.0)
                P.recip(rn[:, h, :], rn[:, h, :])
            for c in range(8):
                P.tt("dve" if c % 2 else "pool", mix[:, c, :], mix[:, c, :], rn[:, c // 2, :], ALU.mult)
            P.dma("sp", s_m, [(mix_dv[:, :, t0:t0 + ST], mix[:])])

        if XPREF:
            load_x(0)
        for st in range(NST):
            t0 = st * ST
            if not XPREF:
                load_x(st)
            rms_to_hT(P, K, xt, gb, hb, hT, pT, identb, ss, sd, junk)
            if XPREF and st + 1 < NST:
                load_x(st + 1)
            P.dma("sp", s_h, [(hT_dv[:, :, t0:t0 + ST], hT[:])])
            w = load_slab(OQ)
            for h in range(4):
                g = gp()
                proj_fm(w, h * 128, g[:])
                P.act(q_fm[:, h, :], g[:], AF.Copy, scale=128.0 ** -0.5)
            w = load_slab(OK_)
            for h in range(4):
                g = gp()
                proj_fm(w, h * 128, g[:])
                P.copy("dve", k_fm[:, h, :], g[:])
            for tc in range(4):
                g = gp()
                proj_tm(w, tc, g[:])
                P.copy("dve", k_tm[:, tc, :], g[:])
            for vs in range(2):
                w = load_slab(OV + vs * 512)
                for tc in range(4):
                    g = gp()
                    proj_tm(w, tc, g[:])
                    P.copy("act" if tc % 2 else "dve", v_tm[:, tc, vs * 512:(vs + 1) * 512], g[:])
            g = gp()
            for kc in range(8):
                P.mm(g[0:16, :], wgkl[:, kc, :], hT[:, kc, :], start=(kc == 0), stop=(kc == 7))
            P.copy("act", gkl[0:16, :], g[0:16, :])
            ptask = [0]
            pw = [None]

            def proj_task():
                i = ptask[0]
                ptask[0] += 1
                if i % 4 == 0:
                    pw[0] = load_slab([OG, OG + 512, OG0, OG0 + 512][i // 4])
                g_ = gp()
                proj_fm(pw[0], (i % 4) * 128, g_[:])
                P.copy("dve", gs[:, i, :] if i < 8 else sg8[:, i - 8, :], g_[:])
            if st > 0:
                tail_a()
            for tc in range(4):
                tsl = slice(tc * 128, (tc + 1) * 128)
                g = gp()
                P.mm(g[:], gkl[0:17, tsl], wgk[0:17, :])
                P.act(e1[:], g[:], AF.Exp, scale=-1.0)
                P.act(sp_tm[:, tc, :], e1[:], AF.Ln, bias=1.0)
                proj_task()
                g = gp()
                P.mm(g[:], SL, sp_tm[:, tc, :])
                P.act(e1[:], g[:], AF.Exp, scale=-1.0 / 16.0)
                P.tt("dve", kdec[:, tc, :], k_tm[:, tc, :], e1[:], ALU.mult)
                g = gp()
                g4 = g[:].rearrange("p (h t) -> p h t", h=4)
                for h in range(4):
                    P.mm(g4[:, h, :], sp_tm[:, tc, h * 128:(h + 1) * 128], U)
                P.copy("dve", B_sb[:], g4)
                proj_task()
                P.tt("dve", Dm[:], B_sb[:], B_sb[:, :, 63:64].to_broadcast([128, 4, 128]), ALU.subtract)
                P.act(ea[:], Dm[:], AF.Exp, scale=-1.0 / 16.0)
                P.act(eb[:], Dm[:], AF.Exp, scale=1.0 / 16.0)
                P.act(ec[:], B_sb[:], AF.Exp, scale=-1.0 / 16.0)
                P.tt("dve", qi[:], q_fm[:, :, tsl], ea[:], ALU.mult)
                P.tt("pool", ki[:], k_fm[:, :, tsl], eb[:], ALU.mult)
                P.tt("dve", qd[:], q_fm[:, :, tsl], ec[:], ALU.mult)
                g = gp()
                g4 = g[:].rearrange("p (h t) -> p h t", h=4)
                for h in range(4):
                    P.mm(g4[:, h, :], ki[:, h, :], qi[:, h, :])
                P.tt("dve", attn[:], g4, U.unsqueeze(1).to_broadcast([128, 4, 128]), ALU.mult)
                proj_task()
                for h in range(4):
                    for vs in range(2):
                        vsl = slice(h * 256 + vs * 128, h * 256 + vs * 128 + 128)
                        P.mm(pO[:, h * 2 + vs, :], v_tm[:, tc, vsl], attn[:, h, :], start=True, stop=False)
                        P.mm(pO[:, h * 2 + vs, :], Sb[:, h, vs * 128:(vs + 1) * 128], qd[:, h, :], start=False, stop=True)
                P.copy("act", o_sb[:, :, tsl], pO[:])
                for h in range(4):
                    P.mm(pS[:, h, :], kdec[:, tc, h * 128:(h + 1) * 128], v_tm[:, tc, h * 256:(h + 1) * 256])
                proj_task()
                for h in range(4):
                    P.stt("dve", S[:, h, :], S[:, h, :], ec[:, h, 127:128], pS[:, h, :], ALU.mult, ALU.add)
                P.copy("pool", Sb[:], S[:])
                if st > 0 and tc == 1:
                    tail_b(t0 - ST)
            assert ptask[0] == 16
        tail_a()
        tail_b((NST - 1) * ST)
        P.emit(es)


def make_consts():
    c = np.zeros((3, 128, 128), np.float32)
    c[0] = np.eye(128, dtype=np.float32)
    s = np.arange(128)[:, None]
    t = np.arange(128)[None, :]
    c[1] = (s <= t).astype(np.float32)
    c[2] = (s > t).astype(np.float32)
    return c


def declare(nc, dbg=None):
    K = Ctx()
    K.nc = nc
    nc._sempool = SemPool(nc)
    di = lambda n, s: nc.dram_tensor(n, s, F32, kind="ExternalInput").ap()
    K.x = di("x", [SEQ, D])
    K.p = di("p", [NL, SEQ, 256])
    K.w_in = di("w_in", [NL, D, IN_DIM])
    K.wgk = di("wgk", [NL, 17, 512])
    K.vfm = di("vfm", [NL, 128, VF_N])
    K.vtm = di("vtm", [VT_N, D])
    K.consts = di("consts", [3, 128, 128])
    K.w_conv_out = di("w_conv_out", [NL, D, D])
    K.w_pool = di("w_pool", [NL, 4, 256, 256])
    K.w_out = di("w_out", [NL, D, D])
    K.w_ffn_gate = di("w_ffn_gate", [1, D, FFN])
    K.w_ffn_up = di("w_ffn_up", [1, D, FFN])
    K.w_ffn_down = di("w_ffn_down", [1, FFN, D])
    K.w_router = di("w_router", [1, D, NE])
    K.b_router = di("b_router", [1, NE])
    K.w_moe_gate = di("w_moe_gate", [1, NE, D, EXP])
    K.w_moe_up = di("w_moe_up", [1, NE, D, EXP])
    K.w_moe_down = di("w_moe_down", [1, NE, EXP, D])
    K.w_ple_gate = di("w_ple_gate", [NL, D, D])
    K.w_ple_proj = di("w_ple_proj", [NL, 256, D])
    K.pool_ratio = di("pool_ratio", [4, 16])
    dbg = dbg or ()
    kd = lambda n: "ExternalOutput" if n in dbg else "Internal"
    K.hT_d = nc.dram_tensor("hT_d", [8, 128, SEQ], BF16, kind=kd("hT_d")).ap()
    K.mix_d = nc.dram_tensor("mix_d", [8, 128, SEQ], F32, kind=kd("mix_d")).ap()
    K.dgd = nc.dram_tensor("dgd", [8, 128, 31 * 128], BF16, kind="Internal").ap()
    K.xa = nc.dram_tensor("xa", [SEQ, D], F32, kind=kd("xa")).ap()
    K.xb = nc.dram_tensor("xb", [SEQ, D], F32, kind=kd("xb")).ap()
    K.out = nc.dram_tensor("out", [SEQ, D], F32, kind="ExternalOutput").ap()
    return K


def host_inputs(inp):
    f = lambda a: np.ascontiguousarray(np.asarray(a, dtype=np.float32))
    shared = {}
    for k in ("w_in", "w_conv_out", "w_pool", "w_out", "w_ffn_gate", "w_ffn_up", "w_ffn_down", "w_router",
              "b_router", "w_moe_gate", "w_moe_up", "w_moe_down", "w_ple_gate", "w_ple_proj"):
        shared[k] = f(inp[k])
    shared["wgk"] = f(np.concatenate([inp["w_gk_up"], inp["b_gk"][:, None, :]], axis=1))
    vfm = np.zeros((NL, 128, VF_N), np.float32)
    for i in range(NL):
        vfm[i, :, VF_CW:VF_CW + 248] = inp["conv_w"][i].reshape(31, 8, 128).transpose(2, 1, 0).reshape(128, 248)
        for off, key in ((VF_CB, "conv_b"), (VF_LG, "ln_conv_g"), (VF_LB, "ln_conv_b"), (VF_PS, "pool_scale")):
            vfm[i, :, off:off + 8] = inp[key][i].reshape(8, 128).T
        vfm[i, :, VF_GN:VF_GN + 2] = inp["g_gla_norm"][i].reshape(2, 128).T
    shared["vfm"] = vfm
    shared["vtm"] = f(np.concatenate([inp["g_mix"], inp["g_ffn"], inp["g_ple"], inp["g_final"][None, :]], axis=0))
    shared["consts"] = make_consts()
    pr = np.ones((4, 16), np.float32)
    for gi, w in enumerate((2, 4, 8, 16)):
        tt = np.arange(16)
        pr[gi] = w / np.minimum(tt + 1, w)
    shared["pool_ratio"] = pr
    maps = []
    for b in range(8):
        m = dict(shared)
        m["x"] = f(inp["x"][b])
        m["p"] = f(inp["p"][:, b])
        maps.append(m)
    return maps


def phase_m2(K, li, x_src, x_dst):
    nc = K.nc
    with ExitStack() as es:
        P = Phase(nc, f"m2{li}")
        pre = f"m2{li}_"
        sb = lambda n, s, d: es.enter_context(nc.sbuf_tensor(pre + n, s, d))
        ps = lambda n, s, d: es.enter_context(nc.psum_tensor(pre + n, s, d))
        gps = [ps(f"g{i}", [128, 512], F32) for i in range(6)]
        pstat = [ps(f"st{i}", [128, 512], F32) for i in range(2)]
        gi_ = [0]

        def gp():
            gi_[0] += 1
            return gps[gi_[0] % 6]
        cst = sb("cst", [128, 128], F32)
        identb = sb("identb", [128, 128], BF16)
        onesb = sb("onesb", [128, 128], BF16)
        eps5 = sb("eps5", [128, 1], F32)
        vf = sb("vf", [128, VF_N], F32)
        ratio = sb("ratio", [128, 4, 16], F32)
        wco = sb("wco", [128, 8, 1024], BF16)
        wo = sb("wo", [128, 8, 1024], BF16)
        wpl = sb("wpl", [128, 4, 2, 256], BF16)
        xt = sb("xt", [128, 4, 1024], F32)
        hT = sb("hT", [128, 8, 512], BF16)
        mixb = sb("mixb", [128, 8, 512], BF16)
        mix = sb("mix", [128, 8, 512], F32)
        NB = 4
        wsl = [sb(f"wsl{i}", [128, 8, 512], BF16) for i in range(NB)]
        u_ext = sb("u_ext", [128, 8, 542], BF16)
        dg = [sb(f"dg{i}", [128, 31, 128], BF16) for i in range(2)]
        y = sb("y", [128, 8, 512], F32)
        ybf = [sb(f"ybf{i}", [128, 512], BF16) for i in range(2)]
        ysq = [sb(f"ysq{i}", [128, 512], BF16) for i in range(2)]
        sw = sb("sw", [128, 8, 512], BF16)
        sg1 = sb("sg1", [128, 8, 512], BF16)
        mean = sb("mean", [128, 512], F32)
        msq = sb("msq", [128, 512], F32)
        rstd = sb("rstd", [128, 512], F32)
        puw = [sb(f"puw{i}", [128, 527], F32) for i in range(2)]
        puh = sb("puh", [128, 8, 15], F32)
        ta = sb("ta", [128, 527], F32)
        tb = sb("tb", [128, 527], F32)
        dd = [sb(f"dd{i}", [128, 2, 512], BF16) for i in range(2)]
        sgt = [sb(f"sgt{i}", [128, 512], F32) for i in range(2)]
        tmp = [sb(f"tmp{i}", [128, 512], F32) for i in range(2)]

        s_c = P.stream("c")
        s_cp = P.stream("cp")
        s_x = P.stream("x")
        s_h = P.stream("h")
        s_m = P.stream("m")
        s_w = [P.stream(f"w{i}") for i in range(NB)]
        s_o = P.stream("o")
        P.dma("sp", s_c, [(vf[:], K.vfm[li]), (ratio[:], K.pool_ratio.partition_broadcast(128)),
                          (cst[:], K.consts[0])])
        P.copy("dve", identb[:], cst[:])
        P.memset("dve", onesb[:], 1.0)
        P.memset("dve", eps5[:], 1e-5)
        P.memset("dve", u_ext[:, :, 0:30], 0.0)
        P.memset("pool", puh[:], 0.0)
        s_g = [P.stream("g0"), P.stream("g1")]
        s_gs = P.stream("gs")
        for c in range(8):
            P.tt("pool", dg[c % 2][:], identb[:].unsqueeze(1).to_broadcast([128, 31, 128]),
                 vf[:, VF_CW + c * 31:VF_CW + (c + 1) * 31].unsqueeze(2).to_broadcast([128, 31, 128]), ALU.mult)
            P.dma("sp", s_gs, [(K.dgd[c].rearrange("p (w n) -> p w n", w=31), dg[c % 2][:])])

        w_in = K.w_in[li]
        slab_cols = [OCA, OCG, OCA + 512, OCG + 512, OG1, OG1 + 512, OPL, OG2, OPL + 512, OG2 + 512]
        sq = SlabQ(P, w_in, wsl, s_w, slab_cols * NST, look=LOOK2)

        sq.after = (3, lambda: P.dma("pool", s_cp, [(wco[:], K.w_conv_out[li].rearrange("(k p) n -> p k n", p=128)),
                                                    (wpl[:], K.w_pool[li].rearrange("g (c p) d -> p g c d", p=128)),
                                                    (wo[:], K.w_out[li].rearrange("(k p) n -> p k n", p=128))]))

        def load_slab(c0):
            assert sq.cols[sq.used] == c0
            return sq.get()

        def proj_fm(w, c0, out_ps):
            for kc in range(8):
                P.mm(out_ps, w[:, kc, c0:c0 + 128], hT[:, kc, :], start=(kc == 0), stop=(kc == 7))

        hT_dv = K.hT_d.rearrange("k p t -> p k t")
        mix_dv = K.mix_d.rearrange("c p t -> p c t")
        cw = lambda c, w_: vf[:, VF_CW + c * 31 + w_:VF_CW + c * 31 + w_ + 1]
        col = lambda off, c: vf[:, off + c:off + c + 1]
        k2 = [0]
        P.dma("sp", s_h, [(hT[:], hT_dv[:, :, 0:ST])])
        for st in range(NST):
            t0 = st * ST
            for s in range(2):
                wa = load_slab(OCA + s * 512)
                wg = load_slab(OCG + s * 512)
                for c4 in range(4):
                    c = s * 4 + c4
                    ga = gp()
                    proj_fm(wa, c4 * 128, ga[:])
                    gg = gp()
                    proj_fm(wg, c4 * 128, gg[:])
                    k2[0] += 1
                    sg_ = sgt[k2[0] % 2]
                    P.act(sg_[:], gg[:], AF.Sigmoid)
                    P.tt("dve", u_ext[:, c, 30:542], ga[:], sg_[:], ALU.mult)
            pm, pq = pstat
            for c in range(9):
                if c < 8:
                    dgc = dg[c % 2]
                    P.dma("sp", s_g[c % 2], [(dgc[:], K.dgd[c].rearrange("p (w n) -> p w n", w=31))])
                    gy = gp()
                    for w_ in range(31):
                        P.mm(gy[:], dgc[:, w_, :], u_ext[:, c, w_:w_ + 512], start=(w_ == 0), stop=(w_ == 30))
                    P.act(y[:, c, :], gy[:], AF.Identity, bias=col(VF_CB, c))
                    P.act(ysq[c % 2][:], y[:, c, :], AF.Square)
                    P.copy("dve", ybf[c % 2][:], y[:, c, :])
                if c > 0:
                    cp_ = c - 1
                    P.mm(pm[:], onesb[:], ybf[cp_ % 2][:], start=(cp_ == 0), stop=(cp_ == 7))
                    P.mm(pq[:], onesb[:], ysq[cp_ % 2][:], start=(cp_ == 0), stop=(cp_ == 7))
            for s in range(2):
                wg1 = load_slab(OG1 + s * 512)
                for c4 in range(4):
                    gg = gp()
                    proj_fm(wg1, c4 * 128, gg[:])
                    P.act(sg1[:, s * 4 + c4, :], gg[:], AF.Sigmoid)
            P.copy("act", u_ext[:, :, 0:30], u_ext[:, :, 512:542])
            P.act(mean[:], pm[:], AF.Copy, scale=1.0 / 1024.0)
            P.tt("dve", msq[:], mean[:], mean[:], ALU.mult)
            P.stt("dve", msq[:], pq[:], 1.0 / 1024.0, msq[:], ALU.mult, ALU.subtract)
            P.act(rstd[:], msq[:], AF.Sqrt, bias=eps5[:, 0:1])
            P.recip(rstd[:], rstd[:])
            for c in range(8):
                P.tt("pool", y[:, c, :], y[:, c, :], mean[:], ALU.subtract)
                P.tt("dve", y[:, c, :], y[:, c, :], rstd[:], ALU.mult)
                P.act(sw[:, c, :], y[:, c, :], AF.Silu, bias=col(VF_LB, c), scale=col(VF_LG, c))
            P.dma("sp", s_m, [(mix[:], mix_dv[:, :, t0:t0 + ST])])
            for m in range(8):
                k2[0] += 1
                tm_ = tmp[k2[0] % 2]
                gy = gp()
                for c in range(8):
                    P.mm(gy[:], wco[:, c, m * 128:(m + 1) * 128], sw[:, c, :], start=(c == 0), stop=(c == 7))
                P.tt("dve", tm_[:], gy[:], sg1[:, m, :], ALU.mult)
                P.tt("pool", mix[:, m, :], mix[:, m, :], tm_[:], ALU.add)
            for s in range(2):
                wp = load_slab(OPL + s * 512)
                wg2 = load_slab(OG2 + s * 512)
                for c4 in range(4):
                    c = s * 4 + c4
                    grp = c // 2
                    gpp = gp()
                    proj_fm(wp, c4 * 128, gpp[:])
                    pu = puw[c % 2]
                    P.copy("dve", pu[:, 0:15], puh[:, c, :])
                    P.copy("act", pu[:, 15:527], gpp[:])
                    P.copy("pool", puh[:, c, :], pu[:, 512:527])
                    src = pu
                    dst = ta
                    for lev in range(1, grp + 2):
                        sh = 1 << (lev - 1)
                        lo = (1 << lev) - 1
                        P.tt("pool" if lev % 2 else "dve", dst[:, lo:527], src[:, lo:527], src[:, lo - sh:527 - sh], ALU.add)
                        src = dst
                        dst = tb if dst is ta else ta
                    if st == 0:
                        P.tt("dve", src[:, 15:31], src[:, 15:31], ratio[:, grp, :], ALU.mult)
                    P.stt("dve", dd[grp % 2][:, c % 2, :], src[:, 15:527], 1.0 / float(1 << (grp + 1)), pu[:, 15:527],
                          ALU.mult, ALU.subtract)
                    if c % 2 == 1:
                        for dd_ in range(2):
                            m = grp * 2 + dd_
                            gg = gp()
                            proj_fm(wg2, (m % 4) * 128, gg[:])
                            k2[0] += 1
                            sg_ = sgt[k2[0] % 2]
                            tm_ = tmp[k2[0] % 2]
                            P.act(sg_[:], gg[:], AF.Sigmoid)
                            gy = gp()
                            for cc in range(2):
                                P.mm(gy[:], wpl[:, grp, cc, dd_ * 128:(dd_ + 1) * 128], dd[grp % 2][:, cc, :],
                                     start=(cc == 0), stop=(cc == 1))
                            P.stt("dve", tm_[:], gy[:], col(VF_PS, m), sg_[:], ALU.mult, ALU.mult)
                            P.tt("pool", mix[:, m, :], mix[:, m, :], tm_[:], ALU.add)
            if st + 1 < NST:
                P.dma("sp", s_h, [(hT[:], hT_dv[:, :, t0 + ST:t0 + 2 * ST])])
            P.dma("sp", s_x, [(xt[:], x_src[t0:t0 + ST, :].rearrange("(c p) d -> p c d", p=128))])
            for c in range(8):
                P.copy("act" if c % 2 else "pool", mixb[:, c, :], mix[:, c, :])
            for tc in range(4):
                for ds_ in range(2):
                    g = gp()
                    for c in range(8):
                        P.mm(g[:], mixb[:, c, tc * 128:(tc + 1) * 128], wo[:, c, ds_ * 512:(ds_ + 1) * 512],
                             start=(c == 0), stop=(c == 7))
                    P.tt("dve", xt[:, tc, ds_ * 512:(ds_ + 1) * 512], xt[:, tc, ds_ * 512:(ds_ + 1) * 512], g[:], ALU.add)
            P.dma("sp", s_o, [(x_dst[t0:t0 + ST, :].rearrange("(c p) d -> p c d", p=128), xt[:])])
        P.emit(es)


def phase_ffn(K, x_src, x_dst, ple_li=None):
    nc = K.nc
    with ExitStack() as es:
        P = Phase(nc, "ffn")
        pre = "ffn_"
        sb = lambda n, s, d: es.enter_context(nc.sbuf_tensor(pre + n, s, d))
        ps = lambda n, s, d: es.enter_context(nc.psum_tensor(pre + n, s, d))
        pT = ps("pT", [128, 8, 128], BF16)
        gps = [ps(f"g{i}", [128, 512], F32) for i in range(7)]
        gi_ = [0]

        def gp():
            gi_[0] += 1
            return gps[gi_[0] % 7]
        cst = sb("cst", [128, 128], F32)
        identb = sb("identb", [128, 128], BF16)
        K.eps6 = sb("eps6", [128, 1], F32)
        gb = sb("gb", [128, 1024], F32)
        xts = [sb(f"xt{i}", [128, 4, 1024], F32) for i in range(2)]
        junk = sb("junk", [128, 1024], BF16)
        ss = sb("ss", [128, 4], F32)
        sd = sb("sd", [128, 4], F32)
        hb = sb("hb", [128, 4, 1024], BF16)
        hT = sb("hT", [128, 8, 512], BF16)
        NC_ = FFN // 128
        wd = sb("wd", [128, NC_, 1024], BF16)
        hid = sb("hid", [128, NC_, 512], BF16)
        NB = 3
        wgu = [sb(f"wg{i}", [128, 8, 256], BF16) for i in range(NB)]
        wuu = [sb(f"wu{i}", [128, 8, 256], BF16) for i in range(NB)]
        sl = [sb(f"sl{i}", [128, 512], BF16) for i in range(2)]
        if ple_li is not None:
            gb2 = sb("gb2", [128, 1024], F32)
            wpg = sb("wpg", [128, 8, 1024], BF16)
            wpp = sb("wpp", [128, 2, 1024], BF16)
            pls = [sb(f"pl{i}", [128, 4, 256], BF16) for i in range(2)]
            pTs = sb("pTs", [128, 2, 512], BF16)
            sgp = [sb(f"sgp{i}", [128, 512], F32) for i in range(2)]
            tmpp = [sb(f"tmpp{i}", [128, 512], F32) for i in range(2)]
        s_c = P.stream("c")
        s_cp = P.stream("cp")
        s_x = P.stream("x")
        s_w = [P.stream(f"w{i}") for i in range(NB)]
        s_o = P.stream("o")
        P.dma("sp", s_c, [(cst[:], K.consts[0]), (gb[:], K.vtm[VT_GFFN + 0].partition_broadcast(128))])
        P.copy("dve", identb[:], cst[:])
        P.memset("dve", K.eps6[:], 1e-6)
        if ple_li is not None:
            s_c2 = P.stream("c2")
            s_cp2 = P.stream("cp2")
            s_ps = [P.stream("p0"), P.stream("p1")]
            P.dma("sp", s_c2, [(gb2[:], K.vtm[VT_GPLE + ple_li].partition_broadcast(128))])
            P.dma("pool", s_cp2, [(wpg[:], K.w_ple_gate[ple_li].rearrange("(k p) n -> p k n", p=128)),
                                  (wpp[:], K.w_ple_proj[ple_li].rearrange("(k p) n -> p k n", p=128))])
        nu = [0]
        k2 = [0]
        s_xs = [s_x, P.stream("x1")]

        def load_x(st_):
            P.dma("sp", s_xs[st_ % 2], [(xts[st_ % 2][:], x_src[st_ * ST:(st_ + 1) * ST, :].rearrange("(c p) d -> p c d", p=128))])
        load_x(0)
        for st in range(NST):
            t0 = st * ST
            xt = xts[st % 2]
            if st + 1 < NST:
                load_x(st + 1)
            if ple_li is not None:
                pl = pls[st % 2]
                P.dma("pool", s_ps[st % 2], [(pl[:], K.p[ple_li][t0:t0 + ST, :].rearrange("(c p) d -> p c d", p=128))])
            rms_to_hT(P, K, xt, gb, hb, hT, pT, identb, ss, sd, junk)
            for u in range(NC_ // 2):
                b = nu[0] % NB
                nu[0] += 1
                c0 = u * 256
                P.dma("pool", s_w[b], [(wgu[b][:], K.w_ffn_gate[0][:, c0:c0 + 256].rearrange("(k p) n -> p k n", p=128)),
                                       (wuu[b][:], K.w_ffn_up[0][:, c0:c0 + 256].rearrange("(k p) n -> p k n", p=128))])
                if nu[0] == 3:
                    P.dma("pool", s_cp, [(wd[:, 2 * j:2 * j + 2, :],
                                         K.w_ffn_down[0][256 * j:256 * j + 256, :].rearrange("(c p) n -> p c n", p=128))
                                        for j in range(NC_ // 2)])
                for hc in range(2):
                    c = 2 * u + hc
                    pg = gp()
                    for kc in range(8):
                        P.mm(pg[:], wgu[b][:, kc, hc * 128:(hc + 1) * 128], hT[:, kc, :], start=(kc == 0), stop=(kc == 7))
                    pu = gp()
                    for kc in range(8):
                        P.mm(pu[:], wuu[b][:, kc, hc * 128:(hc + 1) * 128], hT[:, kc, :], start=(kc == 0), stop=(kc == 7))
                    k2[0] += 1
                    s_ = sl[k2[0] % 2]
                    P.act(s_[:], pg[:], AF.Silu)
                    P.tt("dve", hid[:, c, :], pu[:], s_[:], ALU.mult)
            for tc in range(4):
                for ds_ in range(2):
                    g = gp()
                    for c in range(NC_):
                        P.mm(g[:], hid[:, c, tc * 128:(tc + 1) * 128], wd[:, c, ds_ * 512:(ds_ + 1) * 512],
                             start=(c == 0), stop=(c == NC_ - 1))
                    P.tt("dve", xt[:, tc, ds_ * 512:(ds_ + 1) * 512], xt[:, tc, ds_ * 512:(ds_ + 1) * 512], g[:], ALU.add)
            if ple_li is not None:
                rms_to_hT(P, K, xt, gb2, hb, hT, pT, identb, ss, sd, junk)
                for tc in range(4):
                    for pc in range(2):
                        P.transpose(pT[:, tc * 2 + pc, :], pl[:, tc, pc * 128:(pc + 1) * 128], identb[:])
                for tc in range(4):
                    P.copy("act", pTs[:, :, tc * 128:(tc + 1) * 128], pT[:, tc * 2:tc * 2 + 2, :])
                for tc in range(4):
                    tsl = slice(tc * 128, (tc + 1) * 128)
                    for ds_ in range(2):
                        dsl = slice(ds_ * 512, (ds_ + 1) * 512)
                        gg = gp()
                        for kc in range(8):
                            P.mm(gg[:], hT[:, kc, tsl], wpg[:, kc, dsl], start=(kc == 0), stop=(kc == 7))
                        gy = gp()
                        for pc in range(2):
                            P.mm(gy[:], pTs[:, pc, tsl], wpp[:, pc, dsl], start=(pc == 0), stop=(pc == 1))
                        k2[0] += 1
                        sg_ = sgp[k2[0] % 2]
                        tm_ = tmpp[k2[0] % 2]
                        P.act(sg_[:], gg[:], AF.Sigmoid)
                        P.tt("dve", tm_[:], gy[:], sg_[:], ALU.mult)
                        P.tt("pool", xt[:, tc, dsl], xt[:, tc, dsl], tm_[:], ALU.add)
            P.dma("sp", s_o, [(x_dst[t0:t0 + ST, :].rearrange("(c p) d -> p c d", p=128), xt[:])])
        P.emit(es)


def phase_ple(K, li, x_src, x_dst, final):
    nc = K.nc
    with ExitStack() as es:
        P = Phase(nc, f"ple{li}")
        pre = f"ple{li}_"
        sb = lambda n, s, d: es.enter_context(nc.sbuf_tensor(pre + n, s, d))
        ps = lambda n, s, d: es.enter_context(nc.psum_tensor(pre + n, s, d))
        pT = ps("pT", [128, 8, 128], BF16)
        gps = [ps(f"g{i}", [128, 512], F32) for i in range(6)]
        gi_ = [0]

        def gp():
            gi_[0] += 1
            return gps[gi_[0] % 6]
        cst = sb("cst", [128, 128], F32)
        identb = sb("identb", [128, 128], BF16)
        K.eps6 = sb("eps6", [128, 1], F32)
        gb = sb("gb", [128, 1024], F32)
        gfin = sb("gfin", [128, 1024], F32)
        xts = [sb(f"xt{i}", [128, 4, 1024], F32) for i in range(2)]
        ots = [sb(f"ot{i}", [128, 4, 1024], F32) for i in range(2)]
        junk = sb("junk", [128, 1024], BF16)
        ss = sb("ss", [128, 4], F32)
        sd = sb("sd", [128, 4], F32)
        hb = sb("hb", [128, 4, 1024], BF16)
        hT = sb("hT", [128, 8, 512], BF16)
        wpg = sb("wpg", [128, 8, 1024], BF16)
        wpp = sb("wpp", [128, 2, 1024], BF16)
        pls = [sb(f"pl{i}", [128, 4, 256], BF16) for i in range(2)]
        pTs = sb("pTs", [128, 2, 512], BF16)
        sg = [sb(f"sg{i}", [128, 512], F32) for i in range(2)]
        tmp = [sb(f"tmp{i}", [128, 512], F32) for i in range(2)]
        s_c = P.stream("c")
        s_cp = P.stream("cp")
        s_x = P.stream("x")
        s_p = P.stream("p")
        s_o = P.stream("o")
        P.dma("sp", s_c, [(cst[:], K.consts[0]), (gb[:], K.vtm[VT_GPLE + li].partition_broadcast(128)),
                          (gfin[:], K.vtm[VT_GFIN].partition_broadcast(128))])
        P.dma("pool", s_cp, [(wpg[:], K.w_ple_gate[li].rearrange("(k p) n -> p k n", p=128)),
                            (wpp[:], K.w_ple_proj[li].rearrange("(k p) n -> p k n", p=128))])
        P.copy("dve", identb[:], cst[:])
        P.memset("dve", K.eps6[:], 1e-6)
        k2 = [0]
        s_xs = [s_x, P.stream("x1")]
        s_ps = [s_p, P.stream("p1")]
        s_os = [s_o, P.stream("o1")]

        def load_xp(st_):
            sl_ = slice(st_ * ST, (st_ + 1) * ST)
            P.dma("sp", s_xs[st_ % 2], [(xts[st_ % 2][:], x_src[sl_, :].rearrange("(c p) d -> p c d", p=128))])
            P.dma("pool", s_ps[st_ % 2], [(pls[st_ % 2][:], K.p[li][sl_, :].rearrange("(c p) d -> p c d", p=128))])
        load_xp(0)
        for st in range(NST):
            t0 = st * ST
            xt, ot, pl, s_o = xts[st % 2], ots[st % 2], pls[st % 2], s_os[st % 2]
            if st + 1 < NST:
                load_xp(st + 1)
            rms_to_hT(P, K, xt, gb, hb, hT, pT, identb, ss, sd, junk)
            for tc in range(4):
                for pc in range(2):
                    P.transpose(pT[:, tc * 2 + pc, :], pl[:, tc, pc * 128:(pc + 1) * 128], identb[:])
            for tc in range(4):
                P.copy("act", pTs[:, :, tc * 128:(tc + 1) * 128], pT[:, tc * 2:tc * 2 + 2, :])
            for tc in range(4):
                tsl = slice(tc * 128, (tc + 1) * 128)
                for ds_ in range(2):
                    dsl = slice(ds_ * 512, (ds_ + 1) * 512)
                    gg = gp()
                    for kc in range(8):
                        P.mm(gg[:], hT[:, kc, tsl], wpg[:, kc, dsl], start=(kc == 0), stop=(kc == 7))
                    gy = gp()
                    for pc in range(2):
                        P.mm(gy[:], pTs[:, pc, tsl], wpp[:, pc, dsl], start=(pc == 0), stop=(pc == 1))
                    k2[0] += 1
                    sg_ = sg[k2[0] % 2]
                    tm_ = tmp[k2[0] % 2]
                    P.act(sg_[:], gg[:], AF.Sigmoid)
                    P.tt("dve", tm_[:], gy[:], sg_[:], ALU.mult)
                    P.tt("pool", xt[:, tc, dsl], xt[:, tc, dsl], tm_[:], ALU.add)
            if final:
                for tc in range(4):
                    P.act(junk[:], xt[:, tc, :], AF.Square, scale=1.0 / 32.0, accum_out=ss[:, tc:tc + 1])
                P.act(sd[:], ss[:], AF.Sqrt, bias=K.eps6[:, 0:1])
                P.recip(sd[:], sd[:])
                for tc in range(4):
                    P.stt("dve", ot[:, tc, :], xt[:, tc, :], sd[:, tc:tc + 1], gfin[:], ALU.mult, ALU.mult)
                P.dma("sp", s_o, [(x_dst[t0:t0 + ST, :].rearrange("(c p) d -> p c d", p=128), ot[:])])
            else:
                P.dma("sp", s_o, [(x_dst[t0:t0 + ST, :].rearrange("(c p) d -> p c d", p=128), xt[:])])
        P.emit(es)


def phase_moe(K, x_src, x_dst):
    nc = K.nc
    TP = 1024
    NTC = TP // 128
    NPASS = (NST * ST) // TP
    HC = 14
    with ExitStack() as es:
        P = Phase(nc, "moe")
        pre = "moe_"
        sb = lambda n, s, d: es.enter_context(nc.sbuf_tensor(pre + n, s, d))
        ps = lambda n, s, d: es.enter_context(nc.psum_tensor(pre + n, s, d))
        pT = ps("pT", [128, 8, 128], BF16)
        gps = [ps(f"g{i}", [128, 512], F32) for i in range(7)]
        gi_ = [0]

        def gp():
            gi_[0] += 1
            return gps[gi_[0] % 7]
        cst = sb("cst", [128, 128], F32)
        identb = sb("identb", [128, 128], BF16)
        K.eps6 = sb("eps6", [128, 1], F32)
        gb = sb("gb", [128, 1024], F32)
        br = sb("br", [128, NE], F32)
        wr = sb("wr", [128, 8, NE], BF16)
        xts = [sb(f"xt{i}", [128, NTC, 1024], F32) for i in range(2)]
        junk = sb("junk", [128, 1024], BF16)
        ss = sb("ss", [128, NTC], F32)
        sd = sb("sd", [128, NTC], F32)
        hb = sb("hb", [128, 2, 1024], BF16)
        hT = sb("hT", [128, 8, TP], BF16)
        lgt = sb("lgt", [128, NE], F32)
        l2 = sb("l2", [128, NE], F32)
        eq1 = sb("eq1", [128, NE], F32)
        eq2 = sb("eq2", [128, NE], F32)
        m1 = sb("m1", [128, 1], F32)
        m2 = sb("m2", [128, 1], F32)
        dw = sb("dw", [128, 1], F32)
        w1 = sb("w1", [128, 1], F32)
        w2 = sb("w2", [128, 1], F32)
        wts = sb("wts", [128, NTC, NE], F32)
        hid = sb("hid", [128, HC, TP], BF16)
        wd = [sb(f"wd{i}", [128, HC, 1024], BF16) for i in range(2)]
        NB = 3
        wgu = [sb(f"wg{i}", [128, 8, 256], BF16) for i in range(NB)]
        wuu = [sb(f"wu{i}", [128, 8, 256], BF16) for i in range(NB)]
        sl = [sb(f"sl{i}", [128, 512], BF16) for i in range(2)]
        s_c = P.stream("c")
        s_cp = P.stream("cp")
        s_x = P.stream("x")
        s_w = [P.stream(f"w{i}") for i in range(NB)]
        s_d = [P.stream(f"d{i}") for i in range(2)]
        s_o = P.stream("o")
        P.dma("sp", s_c, [(cst[:], K.consts[0]), (gb[:], K.vtm[VT_GFFN + 1].partition_broadcast(128)),
                          (br[:], K.b_router[0].partition_broadcast(128))])
        P.dma("pool", s_cp, [(wr[:], K.w_router[0].rearrange("(k p) e -> p k e", p=128))])
        P.copy("dve", identb[:], cst[:])
        P.memset("dve", K.eps6[:], 1e-6)
        nu = [0]
        nd = [0]
        k2 = [0]
        s_xs = [s_x, P.stream("x1")]

        def load_x(ip_):
            t_ = ip_ * TP
            xt_ = xts[ip_ % 2]
            P.dma("sp", s_xs[ip_ % 2], [(xt_[:, 0:4, :], x_src[t_:t_ + 512, :].rearrange("(c p) d -> p c d", p=128)),
                                        (xt_[:, 4:8, :], x_src[t_ + 512:t_ + 1024, :].rearrange("(c p) d -> p c d", p=128))])
        load_x(0)
        for ip in range(NPASS):
            t0 = ip * TP
            xt = xts[ip % 2]
            if ip + 1 < NPASS:
                load_x(ip + 1)
            rms_to_hT(P, K, xt, gb, hb, hT, pT, identb, ss, sd, junk, ntc=NTC, nhb=2)
            for tc in range(NTC):
                g = gp()
                for kc in range(8):
                    P.mm(g[:, 0:NE], hT[:, kc, tc * 128:(tc + 1) * 128], wr[:, kc, :], start=(kc == 0), stop=(kc == 7))
                P.tt("dve", lgt[:], g[:, 0:NE], br[:], ALU.add)
                P.reduce_max(m1[:], lgt[:])
                P.ts("dve", eq1[:], lgt[:], m1[:, 0:1], None, ALU.is_equal)
                P.stt("dve", l2[:], eq1[:], -1e30, lgt[:], ALU.mult, ALU.add)
                P.reduce_max(m2[:], l2[:])
                P.ts("dve", eq2[:], l2[:], m2[:, 0:1], None, ALU.is_equal)
                P.tt("dve", dw[:], m2[:], m1[:], ALU.subtract)
                P.act(w2[:], dw[:], AF.Sigmoid)
                P.act(w1[:], dw[:], AF.Sigmoid, scale=-1.0)
                P.ts("dve", wts[:, tc, :], eq1[:], w1[:, 0:1], None, ALU.mult)
                P.stt("dve", wts[:, tc, :], eq2[:], w2[:, 0:1], wts[:, tc, :], ALU.mult, ALU.add)
            for e in range(NE):
                for hf in range(2):
                    db = nd[0] % 2
                    nd[0] += 1
                    r0 = hf * HC * 128
                    P.dma("pool", s_d[db], [(wd[db][:, 2 * j:2 * j + 2, :],
                                             K.w_moe_down[0, e][r0 + 256 * j:r0 + 256 * j + 256, :].rearrange("(c p) n -> p c n", p=128))
                                            for j in range(HC // 2)])
                    for u in range(HC // 2):
                        b = nu[0] % NB
                        nu[0] += 1
                        c0 = r0 + u * 256
                        P.dma("pool", s_w[b], [(wgu[b][:], K.w_moe_gate[0, e][:, c0:c0 + 256].rearrange("(k p) n -> p k n", p=128)),
                                               (wuu[b][:], K.w_moe_up[0, e][:, c0:c0 + 256].rearrange("(k p) n -> p k n", p=128))])
                        for hc in range(2):
                            c = 2 * u + hc
                            for sub in range(TP // 512):
                                ssl = slice(sub * 512, (sub + 1) * 512)
                                pg = gp()
                                for kc in range(8):
                                    P.mm(pg[:], wgu[b][:, kc, hc * 128:(hc + 1) * 128], hT[:, kc, ssl], start=(kc == 0), stop=(kc == 7))
                                pu = gp()
                                for kc in range(8):
                                    P.mm(pu[:], wuu[b][:, kc, hc * 128:(hc + 1) * 128], hT[:, kc, ssl], start=(kc == 0), stop=(kc == 7))
                                k2[0] += 1
                                s_ = sl[k2[0] % 2]
                                P.act(s_[:], pg[:], AF.Silu)
                                P.tt("dve", hid[:, c, ssl], pu[:], s_[:], ALU.mult)
                    for tc in range(NTC):
                        for ds_ in range(2):
                            dsl = slice(ds_ * 512, (ds_ + 1) * 512)
                            g = gp()
                            for c in range(HC):
                                P.mm(g[:], hid[:, c, tc * 128:(tc + 1) * 128], wd[db][:, c, dsl], start=(c == 0), stop=(c == HC - 1))
                            P.stt("dve", xt[:, tc, dsl], g[:], wts[:, tc, e:e + 1], xt[:, tc, dsl], ALU.mult, ALU.add)
            P.dma("sp", s_o, [(x_dst[t0:t0 + 512, :].rearrange("(c p) d -> p c d", p=128), xt[:, 0:4, :]),
                              (x_dst[t0 + 512:t0 + 1024, :].rearrange("(c p) d -> p c d", p=128), xt[:, 4:8, :])])
        P.emit(es)


def build_program():
    nc = bass.Bass("TRN2", target_bir_lowering=False)
    K = declare(nc)
    phase_m1(K, 0, K.x)
    phase_m2(K, 0, K.x, K.xa)
    phase_ffn(K, K.xa, K.xb, ple_li=0)
    phase_m1(K, 1, K.xb)
    phase_m2(K, 1, K.xb, K.xa)
    phase_moe(K, K.xa, K.xb)
    phase_ple(K, 1, K.xb, K.out, True)
    nc._sempool.es.close()
    return nc


def kernel(**inputs):
    maps = host_inputs(inputs)
    nc = build_program()
    res = run_bass_kernel_spmd(nc, maps, core_ids=list(range(8)))
    out = np.stack([np.asarray(r["out"], dtype=np.float32) for r in res.results], axis=0)
    return out
```

```python
import numpy as np
from contextlib import ExitStack
import concourse.bass as bass
import concourse.mybir as mybir
from concourse.bass_utils import run_bass_kernel_spmd

F32 = mybir.dt.float32
BF16 = mybir.dt.bfloat16
AF = mybir.ActivationFunctionType
ALU = mybir.AluOpType
AX = mybir.AxisListType

_DT_SIZE = {F32: 4, BF16: 2}
EPOCH = 20000

SEQ = 4096
D = 1024
NL = 2
IN_DIM = 9232
ST = 512
NST = SEQ // ST
FFN = 2816
NE = 8
EXP = 3584
XPREF = True
LOOK = 2
LOOK2 = 2


class _Op:
    __slots__ = ("eng", "idx", "fn", "deps", "signal", "sigcount", "dma", "fill_end", "prefill")

    def __init__(self, eng, idx, fn):
        self.eng = eng
        self.idx = idx
        self.fn = fn
        self.deps = []
        self.signal = False
        self.sigcount = 0
        self.dma = None
        self.fill_end = 0
        self.prefill = 0


class DmaStream:
    def __init__(self, name):
        self.name = name
        self.count = 0
        self.sem = None


class SemPool:
    def __init__(self, nc):
        self.nc = nc
        self.es = ExitStack()
        self.eng = {}
        self.free = {"sp": [], "pool": [], "act": []}
        self.n = 0

    def new(self, tag):
        self.n += 1
        return self.es.enter_context(self.nc.semaphore(f"s{self.n}_{tag}"))

    def eng_sem(self, e, idx):
        lst = self.eng.setdefault(e, [[], 0])[0]
        while len(lst) <= idx:
            lst.append(self.new(e))
        return lst[idx]

    def take(self, q):
        if self.free[q]:
            return self.free[q].pop()
        return [self.new("d" + q), 0]

    def give(self, q, ent):
        self.free[q].append(ent)


class Phase:
    ENGS = ("pe", "act", "dve", "pool", "sp")

    def __init__(self, nc, name="ph"):
        self.nc = nc
        self.name = name
        self.ops = {e: [] for e in self.ENGS}
        self.track = {}
        self.streams = []
        self.notrack = set()
        self.pool = nc._sempool

    def _rng(self, ap):
        name = ap.tensor.name
        apl = ap.ap
        off = ap.offset
        ds = _DT_SIZE.get(ap.dtype, 4)
        space = str(ap.space)
        if space in ("SB", "PSUM"):
            pstep, pcnt = apl[0]
            if pstep == 0:
                p0 = 0
                f0 = off
            else:
                p0 = off // pstep
                f0 = off - p0 * pstep
            ext = 0
            for st, cn in apl[1:]:
                ext += abs(st) * (cn - 1)
            lo, hi = f0 * ds, (f0 + ext + 1) * ds
            if space == "PSUM":
                return name, 0, 128, (lo // 2048) * 2048, ((hi + 2047) // 2048) * 2048
            return name, p0, p0 + pcnt, lo, hi
        ext = 0
        for st, cn in apl:
            ext += abs(st) * (cn - 1)
        return name, 0, 1, off * ds, (off + ext + 1) * ds

    def _access(self, op, ap, write):
        name, p0, p1, lo, hi = self._rng(ap)
        if name in self.notrack:
            return
        recs = self.track.get(name)
        if recs is None:
            recs = self.track[name] = []
        newrecs = []
        for r in recs:
            rp0, rp1, rlo, rhi, rop, rw = r
            if rp1 <= p0 or p1 <= rp0 or rhi <= lo or hi <= rlo:
                newrecs.append(r)
                continue
            if (write or rw) and rop is not op:
                op.deps.append(rop)
            covered = (p0 <= rp0 and rp1 <= p1 and lo <= rlo and rhi <= hi)
            if write and covered:
                continue
            if (not write) and (not rw) and covered and rop.eng == op.eng and rop.dma is None and op.dma is None:
                continue
            newrecs.append(r)
        newrecs.append((p0, p1, lo, hi, op, write))
        self.track[name] = newrecs

    def _add(self, eng, fn, reads, writes):
        lst = self.ops[eng]
        op = _Op(eng, len(lst), fn)
        lst.append(op)
        for ap in reads:
            if ap is not None and not isinstance(ap, (int, float)):
                self._access(op, ap, False)
        for ap in writes:
            self._access(op, ap, True)
        return op

    def mm(self, out, lhsT, rhs, start=True, stop=True):
        return self._add("pe", lambda e: e.matmul(out, lhsT, rhs, start=start, stop=stop), [lhsT, rhs], [out])

    def transpose(self, out, in_, ident):
        return self._add("pe", lambda e: e.transpose(out, in_, ident), [in_, ident], [out])

    def act(self, out, in_, func, bias=None, scale=None, accum_out=None):
        kw = {}
        if bias is not None:
            kw["bias"] = bias
        if scale is not None:
            kw["scale"] = scale
        if accum_out is not None:
            kw["accum_out"] = accum_out
        w = [out] + ([accum_out] if accum_out is not None else [])
        return self._add("act", lambda e: e.activation(out, in_, func, **kw), [in_, bias, scale], w)

    def tt(self, eng, out, in0, in1, op):
        return self._add(eng, lambda e: e.tensor_tensor(out, in0, in1, op), [in0, in1], [out])

    def ts(self, eng, out, in0, s1, s2, op0, op1=None):
        if op1 is None:
            return self._add(eng, lambda e: e.tensor_scalar(out, in0, s1, None, op0), [in0, s1], [out])
        return self._add(eng, lambda e: e.tensor_scalar(out, in0, s1, s2, op0, op1), [in0, s1, s2], [out])

    def stt(self, eng, out, in0, scalar, in1, op0, op1):
        return self._add(eng, lambda e: e.scalar_tensor_tensor(out, in0, scalar, in1, op0, op1),
                         [in0, scalar, in1], [out])

    def copy(self, eng, out, in_):
        if eng == "act":
            return self._add("act", lambda e: e.copy(out, in_), [in_], [out])
        return self._add(eng, lambda e: e.tensor_copy(out, in_), [in_], [out])

    def recip(self, out, in_):
        return self._add("dve", lambda e: e.reciprocal(out, in_), [in_], [out])

    def memset(self, eng, ap, val):
        return self._add(eng, lambda e: e.memset(ap, val), [], [ap])

    def reduce_max(self, out, in_):
        return self._add("dve", lambda e: e.tensor_reduce(out, in_, AX.X, ALU.max), [in_], [out])

    def stream(self, name):
        s = DmaStream(name)
        self.streams.append(s)
        return s

    def dma(self, q, stream, pairs):
        prefill = stream.count
        ops = []
        for (out, in_) in pairs:
            op = self._add(q, (lambda e, out=out, in_=in_: e.dma_start(out, in_)), [in_], [out])
            op.dma = stream
            op.prefill = prefill
            stream.count += 1
            ops.append(op)
        for op in ops:
            op.fill_end = stream.count
        return ops

    def emit(self, es):
        nc = self.nc
        pool = self.pool
        fin = _Op("sp", len(self.ops["sp"]), None)
        self.ops["sp"].append(fin)

        def needs_sem(op, d):
            if d.dma is not None:
                return False
            if d.eng == op.eng and op.eng == "pe":
                return False
            return True

        for e in self.ENGS:
            for op in self.ops[e]:
                for d in op.deps:
                    if needs_sem(op, d):
                        d.signal = True
        for e in self.ENGS:
            ent = pool.eng.setdefault(e, [[], 0])
            c = ent[1]
            for op in self.ops[e]:
                if op.signal and op.dma is None:
                    c += 1
                    op.sigcount = c
            ent[1] = c

        def esem(e, c):
            c0 = c - 1
            return pool.eng_sem(e, c0 // EPOCH), (c0 % EPOCH) + 1
        sq = {}
        for e in self.ENGS:
            for op in self.ops[e]:
                if op.dma is not None:
                    assert sq.setdefault(op.dma, e) == e, "a DMA stream must stay on one queue"
        for st_ in self.streams:
            if st_ in sq:
                st_.ent = pool.take(sq[st_])
                st_.sem = st_.ent[0]
                st_.base = st_.ent[1]
        streams = [st_ for st_ in self.streams if st_ in sq]
        ops_all = self.ops

        def run(ename, eng):
            waited = {}

            def wait(sem, val):
                k = id(sem)
                if waited.get(k, 0) >= val:
                    return
                waited[k] = val
                eng.wait_ge(sem, val)

            for op in ops_all[ename]:
                if op.fn is None:
                    for st_ in streams:
                        if st_.count:
                            wait(st_.sem, 16 * (st_.base + st_.count))
                    continue
                for d in op.deps:
                    if d.dma is not None:
                        wait(d.dma.sem, 16 * (d.dma.base + d.fill_end))
                    elif needs_sem(op, d):
                        wait(*esem(d.eng, d.sigcount))
                if op.dma is not None and op.prefill > 0:
                    wait(op.dma.sem, 16 * (op.dma.base + op.prefill))
                ins = op.fn(eng)
                if op.dma is not None:
                    ins.then_inc(op.dma.sem, 16)
                elif op.signal:
                    ins.then_inc(esem(ename, op.sigcount)[0], 1)

        with nc.Block() as block:
            @block.tensor
            def _(e):
                run("pe", e)

            @block.scalar
            def _(e):
                run("act", e)

            @block.vector
            def _(e):
                run("dve", e)

            @block.gpsimd
            def _(e):
                run("pool", e)

            @block.sync
            def _(e):
                run("sp", e)
        for st_ in streams:
            st_.ent[1] = st_.base + st_.count
            pool.give(sq[st_], st_.ent)


OQ, OK_, OV, OG, OGK, OCA, OCG, OPL, OG0, OG1, OG2 = 0, 512, 1024, 2048, 3072, 3088, 4112, 5136, 6160, 7184, 8208
VF_CW, VF_CB, VF_LG, VF_LB, VF_PS, VF_GN, VF_N = 0, 248, 256, 264, 272, 280, 282
VT_GMIX, VT_GFFN, VT_GPLE, VT_GFIN, VT_N = 0, 2, 4, 6, 7


class Ctx:
    pass


class SlabQ:
    def __init__(self, P, w_in, bufs, streams, cols, look=2):
        self.P, self.w_in, self.bufs, self.streams, self.cols, self.look = P, w_in, bufs, streams, cols, look
        self.issued = 0
        self.used = 0
        self.after = None

    def _issue(self):
        i = self.issued
        b = i % len(self.bufs)
        c0 = self.cols[i]
        self.P.dma("pool", self.streams[b],
                   [(self.bufs[b][:], self.w_in[:, c0:c0 + 512].rearrange("(k p) n -> p k n", p=128))])
        self.issued += 1
        if self.after is not None and self.issued == self.after[0]:
            self.after[1]()

    def get(self):
        while self.issued < len(self.cols) and self.issued <= self.used + self.look:
            self._issue()
        b = self.used % len(self.bufs)
        self.used += 1
        return self.bufs[b]


def rms_to_hT(P, K, xt, gb, hb, hT, pT, identb, ss, sd, junk, ntc=4, nhb=4):
    for tc in range(ntc):
        P.act(junk[:], xt[:, tc, :], AF.Square, scale=1.0 / 32.0, accum_out=ss[:, tc:tc + 1])
    P.act(sd[:, 0:ntc], ss[:, 0:ntc], AF.Sqrt, bias=K.eps6[:, 0:1])
    P.recip(sd[:, 0:ntc], sd[:, 0:ntc])
    for tc in range(ntc):
        hbt = hb[:, tc % nhb, :]
        P.stt("dve", hbt, xt[:, tc, :], sd[:, tc:tc + 1], gb[:], ALU.mult, ALU.mult)
        for kc in range(8):
            P.transpose(pT[:, kc, :], hbt[:, kc * 128:(kc + 1) * 128], identb[:])
        P.copy("act", hT[:, :, tc * 128:(tc + 1) * 128], pT[:])


def phase_m1(K, li, x_src):
    nc = K.nc
    with ExitStack() as es:
        P = Phase(nc, f"m1{li}")
        pre = f"m1{li}_"
        sb = lambda n, s, d: es.enter_context(nc.sbuf_tensor(pre + n, s, d))
        ps = lambda n, s, d: es.enter_context(nc.psum_tensor(pre + n, s, d))
        pO = ps("pO", [128, 8, 128], F32)
        pS = ps("pS", [128, 4, 256], F32)
        pT = ps("pT", [128, 8, 128], BF16)
        gps = [ps(f"g{i}", [128, 512], F32) for i in range(3)]
        gi = [0]

        def gp():
            gi[0] += 1
            return gps[gi[0] % 3]
        cst = sb("cst", [128, 3, 128], F32)
        identb = sb("identb", [128, 128], BF16)
        onesb = sb("onesb", [128, 128], BF16)
        K.eps6 = sb("eps6", [128, 1], F32)
        eps5 = sb("eps5", [128, 1], F32)
        gb = sb("gb", [128, 1024], F32)
        wgk = sb("wgk", [17, 512], BF16)
        wgkl = sb("wgkl", [128, 8, 16], BF16)
        vf = sb("vf", [128, VF_N], F32)
        S = sb("S", [128, 4, 256], F32)
        Sb = sb("Sb", [128, 4, 256], BF16)
        gkl = sb("gkl", [17, 512], BF16)
        xt = sb("xt", [128, 4, 1024], F32)
        junk = sb("junk", [128, 1024], BF16)
        ss = sb("ss", [128, 4], F32)
        sd = sb("sd", [128, 4], F32)
        hb = sb("hb", [128, 4, 1024], BF16)
        hT = sb("hT", [128, 8, 512], BF16)
        NB = 3
        wsl = [sb(f"wsl{i}", [128, 8, 512], BF16) for i in range(NB)]
        q_fm = sb("q_fm", [128, 4, 512], F32)
        k_fm = sb("k_fm", [128, 4, 512], F32)
        k_tm = sb("k_tm", [128, 4, 512], F32)
        v_tm = sb("v_tm", [128, 4, 1024], BF16)
        sp_tm = sb("sp_tm", [128, 4, 512], F32)
        e1 = sb("e1", [128, 512], F32)
        kdec = sb("kdec", [128, 4, 512], BF16)
        B_sb = sb("B_sb", [128, 4, 128], F32)
        Dm = sb("Dm", [128, 4, 128], F32)
        ea = sb("ea", [128, 4, 128], F32)
        eb = sb("eb", [128, 4, 128], F32)
        ec = sb("ec", [128, 4, 128], F32)
        qi = sb("qi", [128, 4, 128], BF16)
        ki = sb("ki", [128, 4, 128], BF16)
        qd = sb("qd", [128, 4, 128], BF16)
        attn = sb("attn", [128, 4, 128], BF16)
        o_sb = sb("o_sb", [128, 8, 512], F32)
        osq = sb("osq", [128, 8, 512], BF16)
        gs = sb("gs", [128, 8, 512], BF16)
        rn = sb("rn", [128, 4, 512], F32)
        t1s = [sb(f"t1{i}", [128, 512], F32) for i in range(2)]
        sg8 = sb("sg8", [128, 8, 512], BF16)
        mix = sb("mix", [128, 8, 512], F32)
        s_c = P.stream("c")
        s_cp = P.stream("cp")
        s_x = P.stream("x")
        s_w = [P.stream(f"w{i}") for i in range(NB)]
        s_h = P.stream("h")
        s_m = P.stream("m")

        ident = cst[:, 0, :]
        U = cst[:, 1, :]
        SL = cst[:, 2, :]
        P.dma("sp", s_c, [(cst[:], K.consts[0:3].rearrange("c p n -> p c n")),
                          (gb[:], K.vtm[VT_GMIX + li].partition_broadcast(128)),
                          (vf[:], K.vfm[li])])
        P.dma("pool", s_cp, [(wgk[:], K.wgk[li]),
                            (wgkl[:], K.w_in[li][:, OGK:OGK + 16].rearrange("(k p) n -> p k n", p=128))])
        P.copy("dve", identb[:], ident)
        P.memset("dve", onesb[:], 1.0)
        P.memset("dve", K.eps6[:], 1e-6)
        P.memset("dve", eps5[:], 1e-5)
        P.memset("dve", S[:], 0.0)
        P.memset("pool", Sb[:], 0.0)
        P.memset("pool", gkl[:], 1.0)

        slab_cols = [OQ, OK_, OV, OV + 512, OG, OG + 512, OG0, OG0 + 512]
        w_in = K.w_in[li]
        sq = SlabQ(P, w_in, wsl, s_w, slab_cols * NST, look=LOOK)

        def load_slab(c0):
            assert sq.cols[sq.used] == c0
            return sq.get()

        def load_x(st_):
            P.dma("sp", s_x, [(xt[:], x_src[st_ * ST:(st_ + 1) * ST, :].rearrange("(c p) d -> p c d", p=128))])

        def proj_fm(w, c0, out_ps):
            for kc in range(8):
                P.mm(out_ps, w[:, kc, c0:c0 + 128], hT[:, kc, :], start=(kc == 0), stop=(kc == 7))

        def proj_tm(w, tc, out_ps, n=512):
            for kc in range(8):
                P.mm(out_ps, hT[:, kc, tc * 128:(tc + 1) * 128], w[:, kc, 0:n], start=(kc == 0), stop=(kc == 7))

        hT_dv = K.hT_d.rearrange("k p t -> p k t")
        mix_dv = K.mix_d.rearrange("c p t -> p c t")
        def tail_a():
            P.act(gs[:], gs[:], AF.Silu)
            P.act(sg8[:], sg8[:], AF.Sigmoid)
            for c in range(8):
                t1_ = t1s[c % 2]
                P.tt("pool", osq[:, c, :], o_sb[:, c, :], o_sb[:, c, :], ALU.mult)
                P.stt("dve", t1_[:], o_sb[:, c, :], vf[:, VF_GN + (c % 2):VF_GN + (c % 2) + 1], gs[:, c, :],
                      ALU.mult, ALU.mult)
                P.tt("pool", mix[:, c, :], t1_[:], sg8[:, c, :], ALU.mult)

        def tail_b(t0):
            for h in range(4):
                g = gp()
                P.mm(g[:], onesb[:], osq[:, 2 * h, :], start=True, stop=False)
                P.mm(g[:], onesb[:], osq[:, 2 * h + 1, :], start=False, stop=True)
                P.act(rn[:, h, :], g[:], AF.Sqrt, bias=eps5[:, 0:1], scale=1.0 / 256.0)
                P.recip(rn[:, h, :], rn[:, h, :])
            for c in range(8):
                P.tt("dve" if c % 2 else "pool", mix[:, c, :], mix[:, c, :], rn[:, c // 2, :], ALU.mult)
            P.dma("sp", s_m, [(mix_dv[:, :, t0:t0 + ST], mix[:])])

        if XPREF:
            load_x(0)
        for st in range(NST):
            t0 = st * ST
            if not XPREF:
                load_x(st)
            rms_to_hT(P, K, xt, gb, hb, hT, pT, identb, ss, sd, junk)
            if XPREF and st + 1 < NST:
                load_x(st + 1)
            P.dma("sp", s_h, [(hT_dv[:, :, t0:t0 + ST], hT[:])])
            w = load_slab(OQ)
            for h in range(4):
                g = gp()
                proj_fm(w, h * 128, g[:])
                P.act(q_fm[:, h, :], g[:], AF.Copy, scale=128.0 ** -0.5)
            w = load_slab(OK_)
            for h in range(4):
                g = gp()
                proj_fm(w, h * 128, g[:])
                P.copy("dve", k_fm[:, h, :], g[:])
            for tc in range(4):
                g = gp()
                proj_tm(w, tc, g[:])
                P.copy("dve", k_tm[:, tc, :], g[:])
            for vs in range(2):
                w = load_slab(OV + vs * 512)
                for tc in range(4):
                    g = gp()
                    proj_tm(w, tc, g[:])
                    P.copy("act" if tc % 2 else "dve", v_tm[:, tc, vs * 512:(vs + 1) * 512], g[:])
            g = gp()
            for kc in range(8):
                P.mm(g[0:16, :], wgkl[:, kc, :], hT[:, kc, :], start=(kc == 0), stop=(kc == 7))
            P.copy("act", gkl[0:16, :], g[0:16, :])
            ptask = [0]
            pw = [None]

            def proj_task():
                i = ptask[0]
                ptask[0] += 1
                if i % 4 == 0:
                    pw[0] = load_slab([OG, OG + 512, OG0, OG0 + 512][i // 4])
                g_ = gp()
                proj_fm(pw[0], (i % 4) * 128, g_[:])
                P.copy("dve", gs[:, i, :] if i < 8 else sg8[:, i - 8, :], g_[:])
            if st > 0:
                tail_a()
            for tc in range(4):
                tsl = slice(tc * 128, (tc + 1) * 128)
                g = gp()
                P.mm(g[:], gkl[0:17, tsl], wgk[0:17, :])
                P.act(e1[:], g[:], AF.Exp, scale=-1.0)
                P.act(sp_tm[:, tc, :], e1[:], AF.Ln, bias=1.0)
                proj_task()
                g = gp()
                P.mm(g[:], SL, sp_tm[:, tc, :])
                P.act(e1[:], g[:], AF.Exp, scale=-1.0 / 16.0)
                P.tt("dve", kdec[:, tc, :], k_tm[:, tc, :], e1[:], ALU.mult)
                g = gp()
                g4 = g[:].rearrange("p (h t) -> p h t", h=4)
                for h in range(4):
                    P.mm(g4[:, h, :], sp_tm[:, tc, h * 128:(h + 1) * 128], U)
                P.copy("dve", B_sb[:], g4)
                proj_task()
                P.tt("dve", Dm[:], B_sb[:], B_sb[:, :, 63:64].to_broadcast([128, 4, 128]), ALU.subtract)
                P.act(ea[:], Dm[:], AF.Exp, scale=-1.0 / 16.0)
                P.act(eb[:], Dm[:], AF.Exp, scale=1.0 / 16.0)
                P.act(ec[:], B_sb[:], AF.Exp, scale=-1.0 / 16.0)
                P.tt("dve", qi[:], q_fm[:, :, tsl], ea[:], ALU.mult)
                P.tt("pool", ki[:], k_fm[:, :, tsl], eb[:], ALU.mult)
                P.tt("dve", qd[:], q_fm[:, :, tsl], ec[:], ALU.mult)
                g = gp()
                g4 = g[:].rearrange("p (h t) -> p h t", h=4)
                for h in range(4):
                    P.mm(g4[:, h, :], ki[:, h, :], qi[:, h, :])
                P.tt("dve", attn[:], g4, U.unsqueeze(1).to_broadcast([128, 4, 128]), ALU.mult)
                proj_task()
                for h in range(4):
                    for vs in range(2):
                        vsl = slice(h * 256 + vs * 128, h * 256 + vs * 128 + 128)
                        P.mm(pO[:, h * 2 + vs, :], v_tm[:, tc, vsl], attn[:, h, :], start=True, stop=False)
                        P.mm(pO[:, h * 2 + vs, :], Sb[:, h, vs * 128:(vs + 1) * 128], qd[:, h, :], start=False, stop=True)
                P.copy("act", o_sb[:, :, tsl], pO[:])
                for h in range(4):
                    P.mm(pS[:, h, :], kdec[:, tc, h * 128:(h + 1) * 128], v_tm[:, tc, h * 256:(h + 1) * 256])
                proj_task()
                for h in range(4):
                    P.stt("dve", S[:, h, :], S[:, h, :], ec[:, h, 127:128], pS[:, h, :], ALU.mult, ALU.add)
                P.copy("pool", Sb[:], S[:])
                if st > 0 and tc == 1:
                    tail_b(t0 - ST)
            assert ptask[0] == 16
        tail_a()
        tail_b((NST - 1) * ST)
        P.emit(es)


def make_consts():
    c = np.zeros((3, 128, 128), np.float32)
    c[0] = np.eye(128, dtype=np.float32)
    s = np.arange(128)[:, None]
    t = np.arange(128)[None, :]
    c[1] = (s <= t).astype(np.float32)
    c[2] = (s > t).astype(np.float32)
    return c


def declare(nc, dbg=None):
    K = Ctx()
    K.nc = nc
    nc._sempool = SemPool(nc)
    di = lambda n, s: nc.dram_tensor(n, s, F32, kind="ExternalInput").ap()
    K.x = di("x", [SEQ, D])
    K.p = di("p", [NL, SEQ, 256])
    K.w_in = di("w_in", [NL, D, IN_DIM])
    K.wgk = di("wgk", [NL, 17, 512])
    K.vfm = di("vfm", [NL, 128, VF_N])
    K.vtm = di("vtm", [VT_N, D])
    K.consts = di("consts", [3, 128, 128])
    K.w_conv_out = di("w_conv_out", [NL, D, D])
    K.w_pool = di("w_pool", [NL, 4, 256, 256])
    K.w_out = di("w_out", [NL, D, D])
    K.w_ffn_gate = di("w_ffn_gate", [1, D, FFN])
    K.w_ffn_up = di("w_ffn_up", [1, D, FFN])
    K.w_ffn_down = di("w_ffn_down", [1, FFN, D])
    K.w_router = di("w_router", [1, D, NE])
    K.b_router = di("b_router", [1, NE])
    K.w_moe_gate = di("w_moe_gate", [1, NE, D, EXP])
    K.w_moe_up = di("w_moe_up", [1, NE, D, EXP])
    K.w_moe_down = di("w_moe_down", [1, NE, EXP, D])
    K.w_ple_gate = di("w_ple_gate", [NL, D, D])
    K.w_ple_proj = di("w_ple_proj", [NL, 256, D])
    K.pool_ratio = di("pool_ratio", [4, 16])
    dbg = dbg or ()
    kd = lambda n: "ExternalOutput" if n in dbg else "Internal"
    K.hT_d = nc.dram_tensor("hT_d", [8, 128, SEQ], BF16, kind=kd("hT_d")).ap()
    K.mix_d = nc.dram_tensor("mix_d", [8, 128, SEQ], F32, kind=kd("mix_d")).ap()
    K.dgd = nc.dram_tensor("dgd", [8, 128, 31 * 128], BF16, kind="Internal").ap()
    K.xa = nc.dram_tensor("xa", [SEQ, D], F32, kind=kd("xa")).ap()
    K.xb = nc.dram_tensor("xb", [SEQ, D], F32, kind=kd("xb")).ap()
    K.out = nc.dram_tensor("out", [SEQ, D], F32, kind="ExternalOutput").ap()
    return K


def host_inputs(inp):
    f = lambda a: np.ascontiguousarray(np.asarray(a, dtype=np.float32))
    shared = {}
    for k in ("w_in", "w_conv_out", "w_pool", "w_out", "w_ffn_gate", "w_ffn_up", "w_ffn_down", "w_router",
              "b_router", "w_moe_gate", "w_moe_up", "w_moe_down", "w_ple_gate", "w_ple_proj"):
        shared[k] = f(inp[k])
    shared["wgk"] = f(np.concatenate([inp["w_gk_up"], inp["b_gk"][:, None, :]], axis=1))
    vfm = np.zeros((NL, 128, VF_N), np.float32)
    for i in range(NL):
        vfm[i, :, VF_CW:VF_CW + 248] = inp["conv_w"][i].reshape(31, 8, 128).transpose(2, 1, 0).reshape(128, 248)
        for off, key in ((VF_CB, "conv_b"), (VF_LG, "ln_conv_g"), (VF_LB, "ln_conv_b"), (VF_PS, "pool_scale")):
            vfm[i, :, off:off + 8] = inp[key][i].reshape(8, 128).T
        vfm[i, :, VF_GN:VF_GN + 2] = inp["g_gla_norm"][i].reshape(2, 128).T
    shared["vfm"] = vfm
    shared["vtm"] = f(np.concatenate([inp["g_mix"], inp["g_ffn"], inp["g_ple"], inp["g_final"][None, :]], axis=0))
    shared["consts"] = make_consts()
    pr = np.ones((4, 16), np.float32)
    for gi, w in enumerate((2, 4, 8, 16)):
        tt = np.arange(16)
        pr[gi] = w / np.minimum(tt + 1, w)
    shared["pool_ratio"] = pr
    maps = []
    for b in range(8):
        m = dict(shared)
        m["x"] = f(inp["x"][b])
        m["p"] = f(inp["p"][:, b])
        maps.append(m)
    return maps


def phase_m2(K, li, x_src, x_dst):
    nc = K.nc
    with ExitStack() as es:
        P = Phase(nc, f"m2{li}")
        pre = f"m2{li}_"
        sb = lambda n, s, d: es.enter_context(nc.sbuf_tensor(pre + n, s, d))
        ps = lambda n, s, d: es.enter_context(nc.psum_tensor(pre + n, s, d))
        gps = [ps(f"g{i}", [128, 512], F32) for i in range(6)]
        pstat = [ps(f"st{i}", [128, 512], F32) for i in range(2)]
        gi_ = [0]

        def gp():
            gi_[0] += 1
            return gps[gi_[0] % 6]
        cst = sb("cst", [128, 128], F32)
        identb = sb("identb", [128, 128], BF16)
        onesb = sb("onesb", [128, 128], BF16)
        eps5 = sb("eps5", [128, 1], F32)
        vf = sb("vf", [128, VF_N], F32)
        ratio = sb("ratio", [128, 4, 16], F32)
        wco = sb("wco", [128, 8, 1024], BF16)
        wo = sb("wo", [128, 8, 1024], BF16)
        wpl = sb("wpl", [128, 4, 2, 256], BF16)
        xt = sb("xt", [128, 4, 1024], F32)
        hT = sb("hT", [128, 8, 512], BF16)
        mixb = sb("mixb", [128, 8, 512], BF16)
        mix = sb("mix", [128, 8, 512], F32)
        NB = 4
        wsl = [sb(f"wsl{i}", [128, 8, 512], BF16) for i in range(NB)]
        u_ext = sb("u_ext", [128, 8, 542], BF16)
        dg = [sb(f"dg{i}", [128, 31, 128], BF16) for i in range(2)]
        y = sb("y", [128, 8, 512], F32)
        ybf = [sb(f"ybf{i}", [128, 512], BF16) for i in range(2)]
        ysq = [sb(f"ysq{i}", [128, 512], BF16) for i in range(2)]
        sw = sb("sw", [128, 8, 512], BF16)
        sg1 = sb("sg1", [128, 8, 512], BF16)
        mean = sb("mean", [128, 512], F32)
        msq = sb("msq", [128, 512], F32)
        rstd = sb("rstd", [128, 512], F32)
        puw = [sb(f"puw{i}", [128, 527], F32) for i in range(2)]
        puh = sb("puh", [128, 8, 15], F32)
        ta = sb("ta", [128, 527], F32)
        tb = sb("tb", [128, 527], F32)
        dd = [sb(f"dd{i}", [128, 2, 512], BF16) for i in range(2)]
        sgt = [sb(f"sgt{i}", [128, 512], F32) for i in range(2)]
        tmp = [sb(f"tmp{i}", [128, 512], F32) for i in range(2)]

        s_c = P.stream("c")
        s_cp = P.stream("cp")
        s_x = P.stream("x")
        s_h = P.stream("h")
        s_m = P.stream("m")
        s_w = [P.stream(f"w{i}") for i in range(NB)]
        s_o = P.stream("o")
        P.dma("sp", s_c, [(vf[:], K.vfm[li]), (ratio[:], K.pool_ratio.partition_broadcast(128)),
                          (cst[:], K.consts[0])])
        P.copy("dve", identb[:], cst[:])
        P.memset("dve", onesb[:], 1.0)
        P.memset("dve", eps5[:], 1e-5)
        P.memset("dve", u_ext[:, :, 0:30], 0.0)
        P.memset("pool", puh[:], 0.0)
        s_g = [P.stream("g0"), P.stream("g1")]
        s_gs = P.stream("gs")
        for c in range(8):
            P.tt("pool", dg[c % 2][:], identb[:].unsqueeze(1).to_broadcast([128, 31, 128]),
                 vf[:, VF_CW + c * 31:VF_CW + (c + 1) * 31].unsqueeze(2).to_broadcast([128, 31, 128]), ALU.mult)
            P.dma("sp", s_gs, [(K.dgd[c].rearrange("p (w n) -> p w n", w=31), dg[c % 2][:])])

        w_in = K.w_in[li]
        slab_cols = [OCA, OCG, OCA + 512, OCG + 512, OG1, OG1 + 512, OPL, OG2, OPL + 512, OG2 + 512]
        sq = SlabQ(P, w_in, wsl, s_w, slab_cols * NST, look=LOOK2)

        sq.after = (3, lambda: P.dma("pool", s_cp, [(wco[:], K.w_conv_out[li].rearrange("(k p) n -> p k n", p=128)),
                                                    (wpl[:], K.w_pool[li].rearrange("g (c p) d -> p g c d", p=128)),
                                                    (wo[:], K.w_out[li].rearrange("(k p) n -> p k n", p=128))]))

        def load_slab(c0):
            assert sq.cols[sq.used] == c0
            return sq.get()

        def proj_fm(w, c0, out_ps):
            for kc in range(8):
                P.mm(out_ps, w[:, kc, c0:c0 + 128], hT[:, kc, :], start=(kc == 0), stop=(kc == 7))

        hT_dv = K.hT_d.rearrange("k p t -> p k t")
        mix_dv = K.mix_d.rearrange("c p t -> p c t")
        cw = lambda c, w_: vf[:, VF_CW + c * 31 + w_:VF_CW + c * 31 + w_ + 1]
        col = lambda off, c: vf[:, off + c:off + c + 1]
        k2 = [0]
        P.dma("sp", s_h, [(hT[:], hT_dv[:, :, 0:ST])])
        for st in range(NST):
            t0 = st * ST
            for s in range(2):
                wa = load_slab(OCA + s * 512)
                wg = load_slab(OCG + s * 512)
                for c4 in range(4):
                    c = s * 4 + c4
                    ga = gp()
                    proj_fm(wa, c4 * 128, ga[:])
                    gg = gp()
                    proj_fm(wg, c4 * 128, gg[:])
                    k2[0] += 1
                    sg_ = sgt[k2[0] % 2]
                    P.act(sg_[:], gg[:], AF.Sigmoid)
                    P.tt("dve", u_ext[:, c, 30:542], ga[:], sg_[:], ALU.mult)
            pm, pq = pstat
            for c in range(9):
                if c < 8:
                    dgc = dg[c % 2]
                    P.dma("sp", s_g[c % 2], [(dgc[:], K.dgd[c].rearrange("p (w n) -> p w n", w=31))])
                    gy = gp()
                    for w_ in range(31):
                        P.mm(gy[:], dgc[:, w_, :], u_ext[:, c, w_:w_ + 512], start=(w_ == 0), stop=(w_ == 30))
                    P.act(y[:, c, :], gy[:], AF.Identity, bias=col(VF_CB, c))
                    P.act(ysq[c % 2][:], y[:, c, :], AF.Square)
                    P.copy("dve", ybf[c % 2][:], y[:, c, :])
                if c > 0:
                    cp_ = c - 1
                    P.mm(pm[:], onesb[:], ybf[cp_ % 2][:], start=(cp_ == 0), stop=(cp_ == 7))
                    P.mm(pq[:], onesb[:], ysq[cp_ % 2][:], start=(cp_ == 0), stop=(cp_ == 7))
            for s in range(2):
                wg1 = load_slab(OG1 + s * 512)
                for c4 in range(4):
                    gg = gp()
                    proj_fm(wg1, c4 * 128, gg[:])
                    P.act(sg1[:, s * 4 + c4, :], gg[:], AF.Sigmoid)
            P.copy("act", u_ext[:, :, 0:30], u_ext[:, :, 512:542])
            P.act(mean[:], pm[:], AF.Copy, scale=1.0 / 1024.0)
            P.tt("dve", msq[:], mean[:], mean[:], ALU.mult)
            P.stt("dve", msq[:], pq[:], 1.0 / 1024.0, msq[:], ALU.mult, ALU.subtract)
            P.act(rstd[:], msq[:], AF.Sqrt, bias=eps5[:, 0:1])
            P.recip(rstd[:], rstd[:])
            for c in range(8):
                P.tt("pool", y[:, c, :], y[:, c, :], mean[:], ALU.subtract)
                P.tt("dve", y[:, c, :], y[:, c, :], rstd[:], ALU.mult)
                P.act(sw[:, c, :], y[:, c, :], AF.Silu, bias=col(VF_LB, c), scale=col(VF_LG, c))
            P.dma("sp", s_m, [(mix[:], mix_dv[:, :, t0:t0 + ST])])
            for m in range(8):
                k2[0] += 1
                tm_ = tmp[k2[0] % 2]
                gy = gp()
                for c in range(8):
                    P.mm(gy[:], wco[:, c, m * 128:(m + 1) * 128], sw[:, c, :], start=(c == 0), stop=(c == 7))
                P.tt("dve", tm_[:], gy[:], sg1[:, m, :], ALU.mult)
                P.tt("pool", mix[:, m, :], mix[:, m, :], tm_[:], ALU.add)
            pend = []
            for s in range(2):
                wp = load_slab(OPL + s * 512)
                wg2 = load_slab(OG2 + s * 512)
                for c4 in range(4):
                    c = s * 4 + c4
                    grp = c // 2
                    gpp = gp()
                    proj_fm(wp, c4 * 128, gpp[:])
                    pu = puw[c % 2]
                    P.copy("dve", pu[:, 0:15], puh[:, c, :])
                    P.copy("act", pu[:, 15:527], gpp[:])
                    P.copy("pool", puh[:, c, :], pu[:, 512:527])
                    src = pu
                    dst = ta
                    for lev in range(1, grp + 2):
                        sh = 1 << (lev - 1)
                        lo = (1 << lev) - 1
                        P.tt("pool" if lev % 2 else "dve", dst[:, lo:527], src[:, lo:527], src[:, lo - sh:527 - sh], ALU.add)
                        src = dst
                        dst = tb if dst is ta else ta
                    if st == 0:
                        P.tt("dve", src[:, 15:31], src[:, 15:31], ratio[:, grp, :], ALU.mult)
                    P.stt("dve", dd[grp % 2][:, c % 2, :], src[:, 15:527], 1.0 / float(1 << (grp + 1)), pu[:, 15:527],
                          ALU.mult, ALU.subtract)
                    if c % 2 == 1:
                        for dd_ in range(2):
                            m = grp * 2 + dd_
                            gg = gp()
                            proj_fm(wg2, (m % 4) * 128, gg[:])
                            P.act(sg1[:, m, :], gg[:], AF.Sigmoid)

                        def lin(grp=grp):
                            for dd_ in range(2):
                                m = grp * 2 + dd_
                                k2[0] += 1
                                tm_ = tmp[k2[0] % 2]
                                gy = gp()
                                for cc in range(2):
                                    P.mm(gy[:], wpl[:, grp, cc, dd_ * 128:(dd_ + 1) * 128], dd[grp % 2][:, cc, :],
                                         start=(cc == 0), stop=(cc == 1))
                                P.stt("dve", tm_[:], gy[:], col(VF_PS, m), sg1[:, m, :], ALU.mult, ALU.mult)
                                P.tt("pool", mix[:, m, :], mix[:, m, :], tm_[:], ALU.add)
                        pend.append(lin)
                        if len(pend) > 1:
                            pend.pop(0)()
            while pend:
                pend.pop(0)()
            if st + 1 < NST:
                P.dma("sp", s_h, [(hT[:], hT_dv[:, :, t0 + ST:t0 + 2 * ST])])
            P.dma("sp", s_x, [(xt[:], x_src[t0:t0 + ST, :].rearrange("(c p) d -> p c d", p=128))])
            for c in range(8):
                P.copy("act" if c % 2 else "pool", mixb[:, c, :], mix[:, c, :])
            for tc in range(4):
                for ds_ in range(2):
                    g = gp()
                    for c in range(8):
                        P.mm(g[:], mixb[:, c, tc * 128:(tc + 1) * 128], wo[:, c, ds_ * 512:(ds_ + 1) * 512],
                             start=(c == 0), stop=(c == 7))
                    P.tt("dve", xt[:, tc, ds_ * 512:(ds_ + 1) * 512], xt[:, tc, ds_ * 512:(ds_ + 1) * 512], g[:], ALU.add)
            P.dma("sp", s_o, [(x_dst[t0:t0 + ST, :].rearrange("(c p) d -> p c d", p=128), xt[:])])
        P.emit(es)


def phase_ffn(K, x_src, x_dst, ple_li=None):
    nc = K.nc
    with ExitStack() as es:
        P = Phase(nc, "ffn")
        pre = "ffn_"
        sb = lambda n, s, d: es.enter_context(nc.sbuf_tensor(pre + n, s, d))
        ps = lambda n, s, d: es.enter_context(nc.psum_tensor(pre + n, s, d))
        pT = ps("pT", [128, 8, 128], BF16)
        gps = [ps(f"g{i}", [128, 512], F32) for i in range(7)]
        gi_ = [0]

        def gp():
            gi_[0] += 1
            return gps[gi_[0] % 7]
        cst = sb("cst", [128, 128], F32)
        identb = sb("identb", [128, 128], BF16)
        K.eps6 = sb("eps6", [128, 1], F32)
        gb = sb("gb", [128, 1024], F32)
        xts = [sb(f"xt{i}", [128, 4, 1024], F32) for i in range(2)]
        junk = sb("junk", [128, 1024], BF16)
        ss = sb("ss", [128, 4], F32)
        sd = sb("sd", [128, 4], F32)
        hb = sb("hb", [128, 4, 1024], BF16)
        hT = sb("hT", [128, 8, 512], BF16)
        NC_ = FFN // 128
        wd = sb("wd", [128, NC_, 1024], BF16)
        hid = sb("hid", [128, NC_, 512], BF16)
        NB = 3
        wgu = [sb(f"wg{i}", [128, 8, 256], BF16) for i in range(NB)]
        wuu = [sb(f"wu{i}", [128, 8, 256], BF16) for i in range(NB)]
        sl = [sb(f"sl{i}", [128, 512], BF16) for i in range(2)]
        if ple_li is not None:
            gb2 = sb("gb2", [128, 1024], F32)
            wpg = sb("wpg", [128, 8, 1024], BF16)
            wpp = sb("wpp", [128, 2, 1024], BF16)
            pls = [sb(f"pl{i}", [128, 4, 256], BF16) for i in range(2)]
            pTs = sb("pTs", [128, 2, 512], BF16)
            sgp = [sb(f"sgp{i}", [128, 512], F32) for i in range(2)]
            tmpp = [sb(f"tmpp{i}", [128, 512], F32) for i in range(2)]
        s_c = P.stream("c")
        s_cp = P.stream("cp")
        s_x = P.stream("x")
        s_w = [P.stream(f"w{i}") for i in range(NB)]
        s_o = P.stream("o")
        P.dma("sp", s_c, [(cst[:], K.consts[0]), (gb[:], K.vtm[VT_GFFN + 0].partition_broadcast(128))])
        P.copy("dve", identb[:], cst[:])
        P.memset("dve", K.eps6[:], 1e-6)
        if ple_li is not None:
            s_c2 = P.stream("c2")
            s_cp2 = P.stream("cp2")
            s_ps = [P.stream("p0"), P.stream("p1")]
            P.dma("sp", s_c2, [(gb2[:], K.vtm[VT_GPLE + ple_li].partition_broadcast(128))])
            P.dma("pool", s_cp2, [(wpg[:], K.w_ple_gate[ple_li].rearrange("(k p) n -> p k n", p=128)),
                                  (wpp[:], K.w_ple_proj[ple_li].rearrange("(k p) n -> p k n", p=128))])
        nu = [0]
        k2 = [0]
        s_xs = [s_x, P.stream("x1")]

        def load_x(st_):
            P.dma("sp", s_xs[st_ % 2], [(xts[st_ % 2][:], x_src[st_ * ST:(st_ + 1) * ST, :].rearrange("(c p) d -> p c d", p=128))])
        load_x(0)
        for st in range(NST):
            t0 = st * ST
            xt = xts[st % 2]
            if st + 1 < NST:
                load_x(st + 1)
            if ple_li is not None:
                pl = pls[st % 2]
                P.dma("pool", s_ps[st % 2], [(pl[:], K.p[ple_li][t0:t0 + ST, :].rearrange("(c p) d -> p c d", p=128))])
            rms_to_hT(P, K, xt, gb, hb, hT, pT, identb, ss, sd, junk)
            for u in range(NC_ // 2):
                b = nu[0] % NB
                nu[0] += 1
                c0 = u * 256
                P.dma("pool", s_w[b], [(wgu[b][:], K.w_ffn_gate[0][:, c0:c0 + 256].rearrange("(k p) n -> p k n", p=128)),
                                       (wuu[b][:], K.w_ffn_up[0][:, c0:c0 + 256].rearrange("(k p) n -> p k n", p=128))])
                if nu[0] == 3:
                    P.dma("pool", s_cp, [(wd[:, 2 * j:2 * j + 2, :],
                                         K.w_ffn_down[0][256 * j:256 * j + 256, :].rearrange("(c p) n -> p c n", p=128))
                                        for j in range(NC_ // 2)])
                for hc in range(2):
                    c = 2 * u + hc
                    pg = gp()
                    for kc in range(8):
                        P.mm(pg[:], wgu[b][:, kc, hc * 128:(hc + 1) * 128], hT[:, kc, :], start=(kc == 0), stop=(kc == 7))
                    pu = gp()
                    for kc in range(8):
                        P.mm(pu[:], wuu[b][:, kc, hc * 128:(hc + 1) * 128], hT[:, kc, :], start=(kc == 0), stop=(kc == 7))
                    k2[0] += 1
                    s_ = sl[k2[0] % 2]
                    P.act(s_[:], pg[:], AF.Silu)
                    P.tt("dve", hid[:, c, :], pu[:], s_[:], ALU.mult)
            for tc in range(4):
                for ds_ in range(2):
                    g = gp()
                    for c in range(NC_):
                        P.mm(g[:], hid[:, c, tc * 128:(tc + 1) * 128], wd[:, c, ds_ * 512:(ds_ + 1) * 512],
                             start=(c == 0), stop=(c == NC_ - 1))
                    P.tt("dve", xt[:, tc, ds_ * 512:(ds_ + 1) * 512], xt[:, tc, ds_ * 512:(ds_ + 1) * 512], g[:], ALU.add)
            if ple_li is not None:
                rms_to_hT(P, K, xt, gb2, hb, hT, pT, identb, ss, sd, junk)
                for tc in range(4):
                    for pc in range(2):
                        P.transpose(pT[:, tc * 2 + pc, :], pl[:, tc, pc * 128:(pc + 1) * 128], identb[:])
                for tc in range(4):
                    P.copy("act", pTs[:, :, tc * 128:(tc + 1) * 128], pT[:, tc * 2:tc * 2 + 2, :])
                for tc in range(4):
                    tsl = slice(tc * 128, (tc + 1) * 128)
                    for ds_ in range(2):
                        dsl = slice(ds_ * 512, (ds_ + 1) * 512)
                        gg = gp()
                        for kc in range(8):
                            P.mm(gg[:], hT[:, kc, tsl], wpg[:, kc, dsl], start=(kc == 0), stop=(kc == 7))
                        gy = gp()
                        for pc in range(2):
                            P.mm(gy[:], pTs[:, pc, tsl], wpp[:, pc, dsl], start=(pc == 0), stop=(pc == 1))
                        k2[0] += 1
                        sg_ = sgp[k2[0] % 2]
                        tm_ = tmpp[k2[0] % 2]
                        P.act(sg_[:], gg[:], AF.Sigmoid)
                        P.tt("dve", tm_[:], gy[:], sg_[:], ALU.mult)
                        P.tt("pool", xt[:, tc, dsl], xt[:, tc, dsl], tm_[:], ALU.add)
            P.dma("sp", s_o, [(x_dst[t0:t0 + ST, :].rearrange("(c p) d -> p c d", p=128), xt[:])])
        P.emit(es)


def phase_ple(K, li, x_src, x_dst, final):
    nc = K.nc
    with ExitStack() as es:
        P = Phase(nc, f"ple{li}")
        pre = f"ple{li}_"
        sb = lambda n, s, d: es.enter_context(nc.sbuf_tensor(pre + n, s, d))
        ps = lambda n, s, d: es.enter_context(nc.psum_tensor(pre + n, s, d))
        pT = ps("pT", [128, 8, 128], BF16)
        gps = [ps(f"g{i}", [128, 512], F32) for i in range(6)]
        gi_ = [0]

        def gp():
            gi_[0] += 1
            return gps[gi_[0] % 6]
        cst = sb("cst", [128, 128], F32)
        identb = sb("identb", [128, 128], BF16)
        K.eps6 = sb("eps6", [128, 1], F32)
        gb = sb("gb", [128, 1024], F32)
        gfin = sb("gfin", [128, 1024], F32)
        xts = [sb(f"xt{i}", [128, 4, 1024], F32) for i in range(2)]
        ots = [sb(f"ot{i}", [128, 4, 1024], F32) for i in range(2)]
        junk = sb("junk", [128, 1024], BF16)
        ss = sb("ss", [128, 4], F32)
        sd = sb("sd", [128, 4], F32)
        hb = sb("hb", [128, 4, 1024], BF16)
        hT = sb("hT", [128, 8, 512], BF16)
        wpg = sb("wpg", [128, 8, 1024], BF16)
        wpp = sb("wpp", [128, 2, 1024], BF16)
        pls = [sb(f"pl{i}", [128, 4, 256], BF16) for i in range(2)]
        pTs = sb("pTs", [128, 2, 512], BF16)
        sg = [sb(f"sg{i}", [128, 512], F32) for i in range(2)]
        tmp = [sb(f"tmp{i}", [128, 512], F32) for i in range(2)]
        s_c = P.stream("c")
        s_cp = P.stream("cp")
        s_x = P.stream("x")
        s_p = P.stream("p")
        s_o = P.stream("o")
        P.dma("sp", s_c, [(cst[:], K.consts[0]), (gb[:], K.vtm[VT_GPLE + li].partition_broadcast(128)),
                          (gfin[:], K.vtm[VT_GFIN].partition_broadcast(128))])
        P.dma("pool", s_cp, [(wpg[:], K.w_ple_gate[li].rearrange("(k p) n -> p k n", p=128)),
                            (wpp[:], K.w_ple_proj[li].rearrange("(k p) n -> p k n", p=128))])
        P.copy("dve", identb[:], cst[:])
        P.memset("dve", K.eps6[:], 1e-6)
        k2 = [0]
        s_xs = [s_x, P.stream("x1")]
        s_ps = [s_p, P.stream("p1")]
        s_os = [s_o, P.stream("o1")]

        def load_xp(st_):
            sl_ = slice(st_ * ST, (st_ + 1) * ST)
            P.dma("sp", s_xs[st_ % 2], [(xts[st_ % 2][:], x_src[sl_, :].rearrange("(c p) d -> p c d", p=128))])
            P.dma("pool", s_ps[st_ % 2], [(pls[st_ % 2][:], K.p[li][sl_, :].rearrange("(c p) d -> p c d", p=128))])
        load_xp(0)
        for st in range(NST):
            t0 = st * ST
            xt, ot, pl, s_o = xts[st % 2], ots[st % 2], pls[st % 2], s_os[st % 2]
            if st + 1 < NST:
                load_xp(st + 1)
            rms_to_hT(P, K, xt, gb, hb, hT, pT, identb, ss, sd, junk)
            for tc in range(4):
                for pc in range(2):
                    P.transpose(pT[:, tc * 2 + pc, :], pl[:, tc, pc * 128:(pc + 1) * 128], identb[:])
            for tc in range(4):
                P.copy("act", pTs[:, :, tc * 128:(tc + 1) * 128], pT[:, tc * 2:tc * 2 + 2, :])
            for tc in range(4):
                tsl = slice(tc * 128, (tc + 1) * 128)
                for ds_ in range(2):
                    dsl = slice(ds_ * 512, (ds_ + 1) * 512)
                    gg = gp()
                    for kc in range(8):
                        P.mm(gg[:], hT[:, kc, tsl], wpg[:, kc, dsl], start=(kc == 0), stop=(kc == 7))
                    gy = gp()
                    for pc in range(2):
                        P.mm(gy[:], pTs[:, pc, tsl], wpp[:, pc, dsl], start=(pc == 0), stop=(pc == 1))
                    k2[0] += 1
                    sg_ = sg[k2[0] % 2]
                    tm_ = tmp[k2[0] % 2]
                    P.act(sg_[:], gg[:], AF.Sigmoid)
                    P.tt("dve", tm_[:], gy[:], sg_[:], ALU.mult)
                    P.tt("pool", xt[:, tc, dsl], xt[:, tc, dsl], tm_[:], ALU.add)
            if final:
                for tc in range(4):
                    P.act(junk[:], xt[:, tc, :], AF.Square, scale=1.0 / 32.0, accum_out=ss[:, tc:tc + 1])
                P.act(sd[:], ss[:], AF.Sqrt, bias=K.eps6[:, 0:1])
                P.recip(sd[:], sd[:])
                for tc in range(4):
                    P.stt("dve", ot[:, tc, :], xt[:, tc, :], sd[:, tc:tc + 1], gfin[:], ALU.mult, ALU.mult)
                P.dma("sp", s_o, [(x_dst[t0:t0 + ST, :].rearrange("(c p) d -> p c d", p=128), ot[:])])
            else:
                P.dma("sp", s_o, [(x_dst[t0:t0 + ST, :].rearrange("(c p) d -> p c d", p=128), xt[:])])
        P.emit(es)


def phase_moe(K, x_src, x_dst):
    nc = K.nc
    TP = 1024
    NTC = TP // 128
    NPASS = (NST * ST) // TP
    HC = 14
    with ExitStack() as es:
        P = Phase(nc, "moe")
        pre = "moe_"
        sb = lambda n, s, d: es.enter_context(nc.sbuf_tensor(pre + n, s, d))
        ps = lambda n, s, d: es.enter_context(nc.psum_tensor(pre + n, s, d))
        pT = ps("pT", [128, 8, 128], BF16)
        gps = [ps(f"g{i}", [128, 512], F32) for i in range(7)]
        gi_ = [0]

        def gp():
            gi_[0] += 1
            return gps[gi_[0] % 7]
        cst = sb("cst", [128, 128], F32)
        identb = sb("identb", [128, 128], BF16)
        K.eps6 = sb("eps6", [128, 1], F32)
        gb = sb("gb", [128, 1024], F32)
        br = sb("br", [128, NE], F32)
        wr = sb("wr", [128, 8, NE], BF16)
        xts = [sb(f"xt{i}", [128, NTC, 1024], F32) for i in range(2)]
        junk = sb("junk", [128, 1024], BF16)
        ss = sb("ss", [128, NTC], F32)
        sd = sb("sd", [128, NTC], F32)
        hb = sb("hb", [128, 2, 1024], BF16)
        hT = sb("hT", [128, 8, TP], BF16)
        lgt = sb("lgt", [128, NE], F32)
        l2 = sb("l2", [128, NE], F32)
        eq1 = sb("eq1", [128, NE], F32)
        eq2 = sb("eq2", [128, NE], F32)
        m1 = sb("m1", [128, 1], F32)
        m2 = sb("m2", [128, 1], F32)
        dw = sb("dw", [128, 1], F32)
        w1 = sb("w1", [128, 1], F32)
        w2 = sb("w2", [128, 1], F32)
        wts = sb("wts", [128, NTC, NE], F32)
        hid = sb("hid", [128, HC, TP], BF16)
        wd = [sb(f"wd{i}", [128, HC, 1024], BF16) for i in range(2)]
        NB = 3
        wgu = [sb(f"wg{i}", [128, 8, 256], BF16) for i in range(NB)]
        wuu = [sb(f"wu{i}", [128, 8, 256], BF16) for i in range(NB)]
        sl = [sb(f"sl{i}", [128, 512], BF16) for i in range(2)]
        s_c = P.stream("c")
        s_cp = P.stream("cp")
        s_x = P.stream("x")
        s_w = [P.stream(f"w{i}") for i in range(NB)]
        s_d = [P.stream(f"d{i}") for i in range(2)]
        s_o = P.stream("o")
        P.dma("sp", s_c, [(cst[:], K.consts[0]), (gb[:], K.vtm[VT_GFFN + 1].partition_broadcast(128)),
                          (br[:], K.b_router[0].partition_broadcast(128))])
        P.dma("pool", s_cp, [(wr[:], K.w_router[0].rearrange("(k p) e -> p k e", p=128))])
        P.copy("dve", identb[:], cst[:])
        P.memset("dve", K.eps6[:], 1e-6)
        nu = [0]
        nd = [0]
        k2 = [0]
        s_xs = [s_x, P.stream("x1")]

        def load_x(ip_):
            t_ = ip_ * TP
            xt_ = xts[ip_ % 2]
            P.dma("sp", s_xs[ip_ % 2], [(xt_[:, 0:4, :], x_src[t_:t_ + 512, :].rearrange("(c p) d -> p c d", p=128)),
                                        (xt_[:, 4:8, :], x_src[t_ + 512:t_ + 1024, :].rearrange("(c p) d -> p c d", p=128))])
        load_x(0)
        for ip in range(NPASS):
            t0 = ip * TP
            xt = xts[ip % 2]
            if ip + 1 < NPASS:
                load_x(ip + 1)
            rms_to_hT(P, K, xt, gb, hb, hT, pT, identb, ss, sd, junk, ntc=NTC, nhb=2)
            for tc in range(NTC):
                g = gp()
                for kc in range(8):
                    P.mm(g[:, 0:NE], hT[:, kc, tc * 128:(tc + 1) * 128], wr[:, kc, :], start=(kc == 0), stop=(kc == 7))
                P.tt("dve", lgt[:], g[:, 0:NE], br[:], ALU.add)
                P.reduce_max(m1[:], lgt[:])
                P.ts("dve", eq1[:], lgt[:], m1[:, 0:1], None, ALU.is_equal)
                P.stt("dve", l2[:], eq1[:], -1e30, lgt[:], ALU.mult, ALU.add)
                P.reduce_max(m2[:], l2[:])
                P.ts("dve", eq2[:], l2[:], m2[:, 0:1], None, ALU.is_equal)
                P.tt("dve", dw[:], m2[:], m1[:], ALU.subtract)
                P.act(w2[:], dw[:], AF.Sigmoid)
                P.act(w1[:], dw[:], AF.Sigmoid, scale=-1.0)
                P.ts("dve", wts[:, tc, :], eq1[:], w1[:, 0:1], None, ALU.mult)
                P.stt("dve", wts[:, tc, :], eq2[:], w2[:, 0:1], wts[:, tc, :], ALU.mult, ALU.add)
            for e in range(NE):
                for hf in range(2):
                    db = nd[0] % 2
                    nd[0] += 1
                    r0 = hf * HC * 128
                    P.dma("pool", s_d[db], [(wd[db][:, 2 * j:2 * j + 2, :],
                                             K.w_moe_down[0, e][r0 + 256 * j:r0 + 256 * j + 256, :].rearrange("(c p) n -> p c n", p=128))
                                            for j in range(HC // 2)])
                    for u in range(HC // 2):
                        b = nu[0] % NB
                        nu[0] += 1
                        c0 = r0 + u * 256
                        P.dma("pool", s_w[b], [(wgu[b][:], K.w_moe_gate[0, e][:, c0:c0 + 256].rearrange("(k p) n -> p k n", p=128)),
                                               (wuu[b][:], K.w_moe_up[0, e][:, c0:c0 + 256].rearrange("(k p) n -> p k n", p=128))])
                        for hc in range(2):
                            c = 2 * u + hc
                            for sub in range(TP // 512):
                                ssl = slice(sub * 512, (sub + 1) * 512)
                                pg = gp()
                                for kc in range(8):
                                    P.mm(pg[:], wgu[b][:, kc, hc * 128:(hc + 1) * 128], hT[:, kc, ssl], start=(kc == 0), stop=(kc == 7))
                                pu = gp()
                                for kc in range(8):
                                    P.mm(pu[:], wuu[b][:, kc, hc * 128:(hc + 1) * 128], hT[:, kc, ssl], start=(kc == 0), stop=(kc == 7))
                                k2[0] += 1
                                s_ = sl[k2[0] % 2]
                                P.act(s_[:], pg[:], AF.Silu)
                                P.tt("dve", hid[:, c, ssl], pu[:], s_[:], ALU.mult)
                    for tc in range(NTC):
                        for ds_ in range(2):
                            dsl = slice(ds_ * 512, (ds_ + 1) * 512)
                            g = gp()
                            for c in range(HC):
                                P.mm(g[:], hid[:, c, tc * 128:(tc + 1) * 128], wd[db][:, c, dsl], start=(c == 0), stop=(c == HC - 1))
                            P.stt("dve", xt[:, tc, dsl], g[:], wts[:, tc, e:e + 1], xt[:, tc, dsl], ALU.mult, ALU.add)
            P.dma("sp", s_o, [(x_dst[t0:t0 + 512, :].rearrange("(c p) d -> p c d", p=128), xt[:, 0:4, :]),
                              (x_dst[t0 + 512:t0 + 1024, :].rearrange("(c p) d -> p c d", p=128), xt[:, 4:8, :])])
        P.emit(es)


def build_program():
    nc = bass.Bass("TRN2", target_bir_lowering=False)
    K = declare(nc)
    phase_m1(K, 0, K.x)
    phase_m2(K, 0, K.x, K.xa)
    phase_ffn(K, K.xa, K.xb, ple_li=0)
    phase_m1(K, 1, K.xb)
    phase_m2(K, 1, K.xb, K.xa)
    phase_moe(K, K.xa, K.xb)
    phase_ple(K, 1, K.xb, K.out, True)
    nc._sempool.es.close()
    return nc


def kernel(**inputs):
    maps = host_inputs(inputs)
    nc = build_program()
    res = run_bass_kernel_spmd(nc, maps, core_ids=list(range(8)))
    out = np.stack([np.asarray(r["out"], dtype=np.float32) for r in res.results], axis=0)
    return out
```
